# Optimizing a Trainium2 kernel written in Bass

```python
import math
import jax
import jax.numpy as jnp
from jax import lax
import numpy as np

D_MODEL = 1024
BATCH = 8
SEQ = 4096
DEPTH = 2

HEAD_DIM = 64
N_HEADS = D_MODEL // HEAD_DIM
N_HEADS_FOX = N_HEADS // 2
N_HEADS_MOBA = N_HEADS - N_HEADS_FOX
N_HEADS_DIL = N_HEADS
ATTN_SCALE = HEAD_DIM ** -0.5
Q_BLOCK = 128
MOBA_BLOCK = 256
MOBA_TOPK = 3
MOBA_QCHUNK = 16
DIL_PATTERNS = ((128, 1), (512, 4), (2048, 16))
NUM_BUCKETS = 32
MAX_DISTANCE = 2048
D_FF = 2816
N_EXPERTS = 8
TOP_K = 2
D_FF_EXPERT = 3584
NORM_EPS = 1e-6
NEG_INF = float("-inf")

kernel_name = "hybrid_fox_moba_dilated_moe_trunk"


def rms_norm(x, g):
    xf = x.astype(jnp.float32)
    y = xf * lax.rsqrt(jnp.mean(xf * xf, axis=-1, keepdims=True) + NORM_EPS)
    return (y * g.astype(jnp.float32)).astype(x.dtype)


def modulate(h, g, shift, scale):
    return rms_norm(h, g) * (1 + scale[:, None, :]) + shift[:, None, :]


def to_heads(t, n_heads):
    b, s, _ = t.shape
    return t.reshape(b, s, n_heads, HEAD_DIM).transpose(0, 2, 1, 3)


def from_heads(t):
    b, h, s, dh = t.shape
    return t.transpose(0, 2, 1, 3).reshape(b, s, h * dh)


def t5_bucket(dist):
    n = jnp.maximum(dist, 0)
    max_exact = NUM_BUCKETS // 2
    nf = jnp.maximum(n, 1).astype(jnp.float32)
    log_ratio = jnp.log(nf / max_exact) / math.log(MAX_DISTANCE / max_exact)
    large = max_exact + (log_ratio * (NUM_BUCKETS - max_exact)).astype(jnp.int32)
    large = jnp.minimum(large, NUM_BUCKETS - 1)
    return jnp.where(n < max_exact, n, large)


def fox_attention(q, k, v, log_f):
    b, h, s, dh = q.shape
    n_blk = s // Q_BLOCK
    cum = jnp.cumsum(log_f, axis=-1)
    q_blocks = jnp.moveaxis(q.reshape(b, h, n_blk, Q_BLOCK, dh), 2, 0)
    c_blocks = jnp.moveaxis(cum.reshape(b, h, n_blk, Q_BLOCK), 2, 0)
    k_pos = jnp.arange(s)

    def one_block(args):
        qi, ci, bi = args
        q_pos = bi * Q_BLOCK + jnp.arange(Q_BLOCK)
        sc = jnp.einsum("bhqd,bhkd->bhqk", qi, k).astype(jnp.float32) * ATTN_SCALE
        sc = sc + ci[..., None] - cum[:, :, None, :]
        sc = jnp.where(k_pos[None, :] <= q_pos[:, None], sc, NEG_INF)
        p = jax.nn.softmax(sc, axis=-1)
        return jnp.einsum("bhqk,bhkd->bhqd", p.astype(v.dtype), v)

    out = lax.map(one_block, (q_blocks, c_blocks, jnp.arange(n_blk)))
    return jnp.moveaxis(out, 0, 2).reshape(b, h, s, dh)


def moba_attention(q, k, v, bias_t):
    b, h, s, dh = q.shape
    n_blk = -(-s // MOBA_BLOCK)
    s_pad = n_blk * MOBA_BLOCK
    pad = ((0, 0), (0, 0), (0, s_pad - s), (0, 0))
    k_blocks = jnp.pad(k, pad).reshape(b, h, n_blk, MOBA_BLOCK, dh)
    v_blocks = jnp.pad(v, pad).reshape(b, h, n_blk, MOBA_BLOCK, dh)
    k_mean = jnp.mean(k_blocks.astype(jnp.float32), axis=3)
    gate = jnp.einsum("bhsd,bhnd->bhsn", q.astype(jnp.float32), k_mean)
    q_blk = jnp.arange(s) // MOBA_BLOCK
    fully_past = jnp.arange(n_blk)[None, :] < q_blk[:, None]
    gate = jnp.where(fully_past, gate, NEG_INF)
    if n_blk < MOBA_TOPK:
        gate = jnp.pad(gate, ((0, 0), (0, 0), (0, 0), (0, MOBA_TOPK - n_blk)), constant_values=NEG_INF)
    top_val, top_idx = lax.top_k(gate, MOBA_TOPK)
    sel_valid = jnp.isfinite(top_val)
    top_idx = jnp.minimum(top_idx, n_blk - 1)

    n_qc = s // MOBA_QCHUNK

    def chunks(t):
        return jnp.moveaxis(t.reshape(b, h, n_qc, MOBA_QCHUNK, *t.shape[3:]), 2, 0)

    b_ix = jnp.arange(b)[:, None, None, None]
    h_ix = jnp.arange(h)[None, :, None, None]
    blk_off = jnp.arange(MOBA_BLOCK)
    n_sel = MOBA_TOPK * MOBA_BLOCK

    def one_chunk(args):
        qc, idx, valid, ci = args
        t_pos = ci * MOBA_QCHUNK + jnp.arange(MOBA_QCHUNK)
        k_sel = k_blocks[b_ix, h_ix, idx]
        v_sel = v_blocks[b_ix, h_ix, idx]
        s_sel = jnp.einsum("bhqd,bhqjnd->bhqjn", qc, k_sel).astype(jnp.float32) * ATTN_SCALE
        dist_sel = t_pos[None, None, :, None, None] - (idx[..., None] * MOBA_BLOCK + blk_off)
        s_sel = s_sel + bias_t[h_ix[..., None], t5_bucket(dist_sel)]
        s_sel = jnp.where(valid[..., None], s_sel, NEG_INF)
        own = (ci * MOBA_QCHUNK) // MOBA_BLOCK
        k_own = lax.dynamic_index_in_dim(k_blocks, own, axis=2, keepdims=False)
        v_own = lax.dynamic_index_in_dim(v_blocks, own, axis=2, keepdims=False)
        s_own = jnp.einsum("bhqd,bhnd->bhqn", qc, k_own).astype(jnp.float32) * ATTN_SCALE
        dist_own = t_pos[:, None] - (own * MOBA_BLOCK + blk_off)[None, :]
        s_own = s_own + bias_t[:, t5_bucket(dist_own)][None]
        s_own = jnp.where(dist_own >= 0, s_own, NEG_INF)
        p = jax.nn.softmax(jnp.concatenate([s_sel.reshape(b, h, MOBA_QCHUNK, n_sel), s_own], axis=-1), axis=-1)
        p_sel = p[..., :n_sel].reshape(b, h, MOBA_QCHUNK, MOBA_TOPK, MOBA_BLOCK)
        p_own = p[..., n_sel:]
        return (jnp.einsum("bhqjn,bhqjnd->bhqd", p_sel.astype(v.dtype), v_sel)
                + jnp.einsum("bhqn,bhnd->bhqd", p_own.astype(v.dtype), v_own))

    out = lax.map(one_chunk, (chunks(q), chunks(top_idx), chunks(sel_valid), jnp.arange(n_qc)))
    return jnp.moveaxis(out, 0, 2).reshape(b, h, s, dh)


def dilated_branch(q, k, v, bias_t, span, dil):
    b, h, s, dh = q.shape
    unit = span * dil
    s_pad = -(-s // unit) * unit
    sub_len = s_pad // dil
    nc = sub_len // span
    pad = ((0, 0), (0, 0), (0, s_pad - s), (0, 0))

    def split(t):
        t = jnp.pad(t, pad).reshape(b, h, sub_len, dil, dh).transpose(0, 1, 3, 2, 4)
        return t.reshape(b, h, dil, nc, span, dh)

    def band(t):
        prev = jnp.pad(t, ((0, 0), (0, 0), (0, 0), (1, 0), (0, 0), (0, 0)))[:, :, :, :-1]
        return jnp.concatenate([prev, t], axis=4)

    def unsplit(t):
        rest = t.shape[5:]
        t = t.reshape(b, h, dil, sub_len, *rest)
        t = jnp.swapaxes(t, 2, 3).reshape(b, h, s_pad, *rest)
        return t[:, :, :s]

    qc = split(q)
    k_band = band(split(k))
    v_band = band(split(v))
    rel = jnp.arange(span)[:, None] + span - jnp.arange(2 * span)[None, :]
    in_band = (rel >= 0) & (rel <= span)
    not_before_start = (jnp.arange(nc)[:, None, None] > 0) | (jnp.arange(2 * span)[None, None, :] >= span)
    mask = in_band[None] & not_before_start
    bias = bias_t[:, t5_bucket(rel * dil)]
    sc = jnp.einsum("bhrcqd,bhrckd->bhrcqk", qc, k_band).astype(jnp.float32) * ATTN_SCALE
    sc = jnp.where(mask, sc + bias[None, :, None, None], NEG_INF)
    m = jnp.max(sc, axis=-1)
    e = jnp.exp(sc - m[..., None])
    l = jnp.sum(e, axis=-1)
    o = jnp.einsum("bhrcqk,bhrckd->bhrcqd", e, v_band.astype(jnp.float32)) / l[..., None]
    return unsplit(m), unsplit(l), unsplit(o)


def dilated_attention(q, k, v, bias_t):
    ms, ls, os_ = [], [], []
    for window, dil in DIL_PATTERNS:
        m, l, o = dilated_branch(q, k, v, bias_t, window // dil, dil)
        ms.append(m)
        ls.append(l)
        os_.append(o)
    m_all = jnp.stack(ms)
    w = jnp.stack(ls) * jnp.exp(m_all - jnp.max(m_all, axis=0))
    o = jnp.sum(w[..., None] * jnp.stack(os_), axis=0) / jnp.sum(w, axis=0)[..., None]
    return o.astype(q.dtype)


def even_mixer(u, w_in, gate_bias, w_out, rel_bias_table):
    d_a = N_HEADS_FOX * HEAD_DIM
    d_b = N_HEADS_MOBA * HEAD_DIM
    cuts = np.cumsum([d_a, d_a, d_a, N_HEADS_FOX, d_b, d_b]).tolist()
    q_a, k_a, v_a, f_a, q_b, k_b, v_b = jnp.split(u @ w_in, cuts, axis=-1)
    log_f = jax.nn.log_sigmoid(f_a.astype(jnp.float32) + gate_bias.astype(jnp.float32)).transpose(0, 2, 1)
    o_fox = fox_attention(to_heads(q_a, N_HEADS_FOX), to_heads(k_a, N_HEADS_FOX),
                          to_heads(v_a, N_HEADS_FOX), log_f)
    bias_moba = rel_bias_table.T[N_HEADS_FOX:]
    o_moba = moba_attention(to_heads(q_b, N_HEADS_MOBA), to_heads(k_b, N_HEADS_MOBA),
                            to_heads(v_b, N_HEADS_MOBA), bias_moba)
    o = from_heads(jnp.concatenate([o_fox, o_moba.astype(o_fox.dtype)], axis=1))
    return o @ w_out


def odd_mixer(u, w_in, w_out, rel_bias_table):
    q, k, v = jnp.split(u @ w_in, 3, axis=-1)
    o = dilated_attention(to_heads(q, N_HEADS_DIL), to_heads(k, N_HEADS_DIL),
                          to_heads(v, N_HEADS_DIL), rel_bias_table.T)
    return from_heads(o) @ w_out


def swiglu(u, w_gate, w_up, w_down):
    return (jax.nn.silu(u @ w_gate) * (u @ w_up)) @ w_down


def moe_swiglu(u, router_w, w_gate, w_up, w_down):
    b, s, d = u.shape
    t = u.reshape(b * s, d)
    logits = (t @ router_w).astype(jnp.float32)
    top_v, top_i = lax.top_k(logits, TOP_K)
    top_p = jax.nn.softmax(top_v, axis=-1)
    gates = jnp.einsum("nk,nke->ne", top_p, jax.nn.one_hot(top_i, N_EXPERTS, dtype=jnp.float32))
    out = jnp.zeros_like(t)
    for e in range(N_EXPERTS):
        out = out + gates[:, e:e + 1].astype(t.dtype) * swiglu(t, w_gate[e], w_up[e], w_down[e])
    return out.reshape(b, s, d)


def setup_inputs(seed: int = 0) -> dict:
    key = jax.random.key(seed)
    ks = jax.random.split(key, 18)
    D = D_MODEL
    n_even = (DEPTH + 1) // 2
    n_odd = DEPTH // 2
    d_attn = N_HEADS * HEAD_DIM
    d_in_even = 3 * N_HEADS_FOX * HEAD_DIM + N_HEADS_FOX + 3 * N_HEADS_MOBA * HEAD_DIM

    def rn(k, shape):
        return jax.random.normal(k, shape, jnp.float32)

    def w(k, shape, fan_in, gain=1.0):
        return (gain * fan_in ** -0.5) * rn(k, shape)

    return {
        "x": rn(ks[0], (BATCH, SEQ, D)),
        "c": rn(ks[1], (BATCH, D)),
        "mod_w": w(ks[2], (DEPTH, D, 6 * D), D, 0.5),
        "mod_b": 0.02 * rn(ks[3], (DEPTH, 6 * D)),
        "norm_g": 1.0 + 0.1 * rn(ks[4], (DEPTH, 4, D)),
        "attn_in_w_even": w(ks[5], (n_even, D, d_in_even), D),
        "fox_gate_bias": 1.0 + 0.5 * rn(ks[6], (n_even, N_HEADS_FOX)),
        "attn_out_w_even": w(ks[7], (n_even, d_attn, D), d_attn),
        "attn_in_w_odd": w(ks[8], (n_odd, D, 3 * N_HEADS_DIL * HEAD_DIM), D),
        "attn_out_w_odd": w(ks[9], (n_odd, N_HEADS_DIL * HEAD_DIM, D), N_HEADS_DIL * HEAD_DIM),
        "rel_bias_table": 0.5 * rn(ks[10], (NUM_BUCKETS, N_HEADS)),
        "ffn_w_gate": w(ks[11], (n_even, D, D_FF), D),
        "ffn_w_up": w(ks[12], (n_even, D, D_FF), D),
        "ffn_w_down": w(ks[13], (n_even, D_FF, D), D_FF),
        "router_w": w(ks[14], (n_odd, D, N_EXPERTS), D),
        "exp_w_gate": w(ks[15], (n_odd, N_EXPERTS, D, D_FF_EXPERT), D),
        "exp_w_up": w(ks[16], (n_odd, N_EXPERTS, D, D_FF_EXPERT), D),
        "exp_w_down": w(ks[17], (n_odd, N_EXPERTS, D_FF_EXPERT, D), D_FF_EXPERT),
    }


def reference(x, c, mod_w, mod_b, norm_g, attn_in_w_even, fox_gate_bias, attn_out_w_even,
              attn_in_w_odd, attn_out_w_odd, rel_bias_table, ffn_w_gate, ffn_w_up, ffn_w_down,
              router_w, exp_w_gate, exp_w_up, exp_w_down):
    mods = jnp.einsum("bd,lde->lbe", jax.nn.silu(c), mod_w) + mod_b[:, None, :]
    h = x
    for layer in range(DEPTH):
        j = layer // 2
        sh1, sc1, g1, sh2, sc2, g2 = jnp.split(mods[layer], 6, axis=-1)
        gains = norm_g[layer]
        u = modulate(h, gains[0], sh1, sc1)
        if layer % 2 == 0:
            y = even_mixer(u, attn_in_w_even[j], fox_gate_bias[j], attn_out_w_even[j], rel_bias_table)
        else:
            y = odd_mixer(u, attn_in_w_odd[j], attn_out_w_odd[j], rel_bias_table)
        h = h + g1[:, None, :] * rms_norm(y, gains[1])
        u = modulate(h, gains[2], sh2, sc2)
        if layer % 2 == 0:
            y = swiglu(u, ffn_w_gate[j], ffn_w_up[j], ffn_w_down[j])
        else:
            y = moe_swiglu(u, router_w[j], exp_w_gate[j], exp_w_up[j], exp_w_down[j])
        h = h + g2[:, None, :] * rms_norm(y, gains[3])
    return h
```

```python
import numpy as np
import ml_dtypes
from contextlib import ExitStack
import concourse.bass as bass
import concourse.mybir as mybir
from concourse.ap import AP
from concourse.bass_utils import run_bass_kernel_spmd

F32 = mybir.dt.float32
BF16 = mybir.dt.bfloat16
ALU = mybir.AluOpType
AF = mybir.ActivationFunctionType
AX = mybir.AxisListType

S = 4096
D = 1024
NT = 8
EPS = 1e-6
LC = 3327
WC = 3200
LD = 383
WD = 256
NEG = -30000.0
DILS = (1, 4, 16)


class Res:
    __slots__ = ("writers", "readers", "prev_readers")

    def __init__(self):
        self.writers = []
        self.readers = []
        self.prev_readers = []


class Lane:
    def __init__(self, sem):
        self.sem = sem
        self.count = 0


class Ins:
    __slots__ = ("eng", "fn", "deps", "sig", "cnt", "lane", "lane_val", "idx")


class Ctx:
    def __init__(self, nc, es):
        self.nc = nc
        self.es = es
        self.engs = {"pe": nc.tensor, "act": nc.scalar, "dve": nc.vector, "pool": nc.gpsimd, "sp": nc.sync}
        self.esem = {k: es.enter_context(nc.semaphore("es_" + k)) for k in ("pe", "act", "dve", "pool")}
        self.ecount = {k: 0 for k in self.esem}
        self.lanes = []
        self.nlane = 0

    def lane(self):
        if getattr(self, "free_lanes", None):
            ln = self.free_lanes.pop()
        else:
            ln = Lane(self.es.enter_context(self.nc.semaphore("ln%d" % self.nlane)))
            self.nlane += 1
            self.lanes.append(ln)
        if not hasattr(self, "phase_lanes"):
            self.phase_lanes = []
        self.phase_lanes.append(ln)
        return ln

    def recycle(self):
        if not hasattr(self, "free_lanes"):
            self.free_lanes = []
        self.free_lanes.extend(getattr(self, "phase_lanes", []))
        self.phase_lanes = []


class Phase:
    def __init__(self, K, name):
        self.K = K
        self.name = name
        self.streams = {k: [] for k in ("pe", "act", "dve", "pool", "sp")}
        self.used_lanes = {}

    def add(self, eng, fn, reads=(), writes=(), lane=None, nowaw=False):
        ins = Ins()
        ins.eng = eng
        ins.fn = fn
        ins.sig = False
        ins.cnt = None
        ins.lane = lane
        ins.lane_val = None
        if lane is not None:
            lane.count += 1
            ins.lane_val = 16 * lane.count
            self.used_lanes[id(lane)] = (lane, eng)
        deps = []
        for r in reads:
            deps.extend(r.writers)
        for w in writes:
            deps.extend(w.readers)
            if not nowaw:
                deps.extend(w.writers)
            else:
                deps.extend(w.prev_readers)
        dd = []
        seen = set()
        for dpi in deps:
            if id(dpi) in seen or dpi is ins:
                continue
            seen.add(id(dpi))
            if dpi.lane is None and dpi.eng == "pe" and eng == "pe":
                continue
            dd.append(dpi)
            if dpi.lane is None:
                dpi.sig = True
        ins.deps = dd
        for r in reads:
            r.readers.append(ins)
        for w in writes:
            if nowaw:
                w.writers.append(ins)
                w.prev_readers = w.prev_readers + w.readers
            else:
                w.writers = [ins]
                w.prev_readers = w.readers
            w.readers = []
        ins.idx = len(self.streams[eng])
        self.streams[eng].append(ins)
        return ins

    def emit(self):
        K = self.K
        nc = K.nc
        for e in ("pe", "act", "dve", "pool"):
            for ins in self.streams[e]:
                if ins.lane is None and ins.sig:
                    K.ecount[e] += 1
                    ins.cnt = K.ecount[e]
        streams = self.streams
        used = list(self.used_lanes.values())

        def run(ename, eng):
            waited = {}
            for ins in streams[ename]:
                need = {}
                for dpi in ins.deps:
                    if dpi.lane is not None:
                        key, val, sem = ("L", id(dpi.lane)), dpi.lane_val, dpi.lane.sem
                    else:
                        key, val, sem = ("E", dpi.eng), dpi.cnt, K.esem[dpi.eng]
                    if val > need.get(key, (0, None))[0]:
                        need[key] = (val, sem)
                for key, (val, sem) in need.items():
                    if waited.get(key, 0) >= val:
                        continue
                    eng.wait_ge(sem, val)
                    waited[key] = val
                bi = ins.fn(eng)
                if ins.lane is not None:
                    bi.then_inc(ins.lane.sem, 16)
                elif ins.sig:
                    bi.then_inc(K.esem[ename], 1)
            for lane, le in used:
                if le == ename:
                    eng.wait_ge(lane.sem, 16 * lane.count)

        with nc.Block() as block:
            @block.tensor
            def _(e):
                run("pe", e)

            @block.scalar
            def _(e):
                run("act", e)

            @block.vector
            def _(e):
                run("dve", e)

            @block.gpsimd
            def _(e):
                run("pool", e)

            @block.sync
            def _(e):
                run("sp", e)
        K.recycle()


class Pool2:
    def __init__(self, tiles):
        self.tiles = tiles
        self.res = [Res() for _ in tiles]
        self.i = 0

    def next(self):
        j = self.i % len(self.tiles)
        self.i += 1
        return self.tiles[j], self.res[j]


def dram_view(t, offset, pattern):
    return AP(t.tensor, offset, pattern)


def _t5_bucket_np(dist):
    n = np.maximum(dist, 0)
    nf = np.maximum(n, 1).astype(np.float32)
    lr = np.log(nf / np.float32(16)) / np.float32(np.log(2048 / 16))
    large = 16 + (lr.astype(np.float32) * np.float32(16)).astype(np.int32)
    large = np.minimum(large, 31)
    return np.where(n < 16, n, large)


def make_consts():
    c = {}
    c["ident_f"] = np.eye(128, dtype=np.float32)
    c["ident_b"] = np.eye(128, dtype=np.float32).astype(ml_dtypes.bfloat16)
    oh = np.zeros((33, LC), np.float32)
    d = np.arange(LC) - 511
    bk = _t5_bucket_np(d)
    for m in range(LC):
        if d[m] >= 0:
            oh[bk[m], m] = 1.0
        else:
            oh[32, m] = 1.0
    c["oh_c"] = oh
    ohd = np.zeros((3, 33, LD + 1), np.float32)
    for g, dil in enumerate(DILS):
        rel = np.arange(LD + 1) - 127
        bk = _t5_bucket_np(rel * dil)
        for m in range(LD + 1):
            if 0 <= rel[m] <= 128:
                ohd[g, bk[m], m] = 1.0
            else:
                ohd[g, 32, m] = 1.0
    c["oh_d"] = ohd
    ind = np.zeros((16, S), np.float32)
    for j in range(16):
        ind[j, j * 256:(j + 1) * 256] = 1.0
    c["ind"] = ind.astype(ml_dtypes.bfloat16)
    c["tri"] = np.triu(np.ones((128, 128), np.float32), k=1).astype(ml_dtypes.bfloat16)
    p = np.arange(128, dtype=np.float32)[:, None]
    k = np.arange(32, dtype=np.float32)[None, :]
    c["c1"] = ((k // 4) * 128 + p) * 4 + (k % 4)
    c["c2"] = np.arange(28, dtype=np.float32)[None, :] * 128 + p
    c["jv"] = np.tile(np.arange(24, dtype=np.float32)[None, :], (128, 1))
    return c


class Prog:
    def __init__(self, stop_after=None, debug=False):
        self.stop_after = stop_after
        self.debug = debug
        self.nc = bass.Bass("TRN2", target_bir_lowering=False)
        self.es = ExitStack()
        self.K = Ctx(self.nc, self.es)
        self.dbg_outputs = []

    def din(self, name, shape, dt=F32):
        return self.nc.dram_tensor(name, list(shape), dt, kind="ExternalInput").ap()

    def dscr(self, name, shape, dt, dbg=False):
        if dbg and self.debug:
            self.dbg_outputs.append(name)
            return self.nc.dram_tensor(name, list(shape), dt, kind="ExternalOutput").ap()
        return self.nc.dram_tensor(name, list(shape), dt, kind="Internal").ap()

    def sb(self, es, name, shape, dt):
        self.uid = getattr(self, "uid", 0) + 1
        return es.enter_context(self.nc.sbuf_tensor("s%d_%s" % (self.uid, name), list(shape), dt))

    def ps(self, es, name, shape=(128, 512), dt=F32):
        self.uid = getattr(self, "uid", 0) + 1
        return es.enter_context(self.nc.psum_tensor("p%d_%s" % (self.uid, name), list(shape), dt))

    def build(self):
        nc = self.nc
        I = {}
        I["x"] = self.din("x", [S, D])
        I["c"] = self.din("c", [8, 128])
        I["mod_w"] = self.din("mod_w", [2, D, 6 * D])
        I["mod_b"] = self.din("mod_b", [96, 128])
        I["norm_g"] = self.din("norm_g", [64, 128])
        I["in_w0"] = self.din("in_w0", [D, 3080])
        I["fgb"] = self.din("fgb", [8, 1])
        I["out_w0"] = self.din("out_w0", [D, D])
        I["in_w1"] = self.din("in_w1", [D, 3072])
        I["out_w1"] = self.din("out_w1", [D, D])
        I["rbt"] = self.din("rbt", [32, 16])
        I["ffn_g"] = self.din("ffn_g", [1, D, 2816])
        I["ffn_u"] = self.din("ffn_u", [1, D, 2816])
        I["ffn_d"] = self.din("ffn_d", [1, 2816, D])
        I["router"] = self.din("router", [D, 8])
        I["exp_g"] = self.din("exp_g", [8, D, 3584])
        I["exp_u"] = self.din("exp_u", [8, D, 3584])
        I["exp_d"] = self.din("exp_d", [8, 3584, D])
        I["ident_f"] = self.din("ident_f", [128, 128])
        I["ident_b"] = self.din("ident_b", [128, 128], BF16)
        I["oh_c"] = self.din("oh_c", [33, LC])
        I["oh_d"] = self.din("oh_d", [3, 33, LD + 1])
        I["ind"] = self.din("ind", [16, S], BF16)
        I["tri"] = self.din("tri", [128, 128], BF16)
        I["c1"] = self.din("c1", [128, 32])
        I["c2"] = self.din("c2", [128, 28])
        I["jv"] = self.din("jv", [128, 24])
        self.I = I
        self.out = nc.dram_tensor("out", [S, D], F32, kind="ExternalOutput").ap()

        Dm = {}
        Dm["hT0"] = self.dscr("hT0", [D, S], F32, dbg=True)
        Dm["hT1"] = self.dscr("hT1", [D, S], F32, dbg=True)
        Dm["hT2"] = self.dscr("hT2", [D, S], F32, dbg=True)
        Dm["hT3"] = self.dscr("hT3", [D, S], F32, dbg=True)
        Dm["qkT"] = self.dscr("qkT", [2048, S], BF16, dbg=True)
        Dm["vaug"] = self.dscr("vaug", [3, 16, 128, 32 * 65], BF16, dbg=True)
        Dm["cparts"] = self.dscr("cparts", [8, 6, S], BF16, dbg=True)
        Dm["oT"] = self.dscr("oT", [D, S], BF16, dbg=True)
        self.spT = self.dscr("spT", [8, S], F32, dbg=True)
        Dm["yT"] = self.dscr("yT", [D, S], F32, dbg=True)
        Dm["gc"] = self.dscr("gc", [9, 128, LC], BF16)
        Dm["gd"] = self.dscr("gd", [16, 3, 128, LD + 1], BF16)
        self.Dm = Dm

        es = self.es
        self.vecs = self.sb(es, "vecs", [128, 2, 6, 8], F32)
        self.ident_f = self.sb(es, "identf", [128, 128], F32)
        self.ident_b = self.sb(es, "identb", [128, 128], BF16)
        self.ones_div = self.sb(es, "onesdiv", [128, 128], BF16)
        self.ones_f = self.sb(es, "onesf", [128, 128], F32)

        phases = [
            ("prep", lambda: self.ph_prep()),
            ("inproj0", lambda: self.ph_inproj(0)),
            ("cum", lambda: self.ph_cum()),
            ("fox", lambda: self.ph_attn01(False)),
            ("moba", lambda: self.ph_attn01(True)),
            ("outproj0", lambda: self.ph_outproj(0)),
            ("ffn0", lambda: self.ph_ffn(0)),
            ("inproj1", lambda: self.ph_inproj(1)),
            ("dil", lambda: self.ph_dil()),
            ("outproj1", lambda: self.ph_outproj(1)),
            ("ffn1", lambda: self.ph_moe()),
        ]
        for name, fn in phases:
            fn()
            if self.stop_after == name:
                break
        self.es.close()
        return nc


def _ph_mm(self, out, lhsT, rhs, start, stop, reads=(), writes=()):
    return self.add("pe", lambda e: e.matmul(out, lhsT, rhs, start=start, stop=stop), reads, writes)


def _ph_tr(self, out, in_, ident, reads=(), writes=()):
    return self.add("pe", lambda e: e.transpose(out, in_, ident), reads, writes)


def _ph_act(self, out, in_, func, reads=(), writes=(), bias=0.0, scale=1.0):
    return self.add("act", lambda e: e.activation(out=out, in_=in_, func=func, bias=bias, scale=scale), reads, writes)


def _ph_dma(self, eng, out, in_, lane, reads=(), writes=(), nowaw=False, **kw):
    return self.add(eng, lambda e: e.dma_start(out=out, in_=in_, **kw), reads, writes, lane=lane, nowaw=nowaw)


def _ph_v(self, eng, meth, *args, reads=(), writes=(), nowaw=False, **kw):
    return self.add(eng, lambda e: getattr(e, meth)(*args, **kw), reads, writes, nowaw=nowaw)


Phase.mm = _ph_mm
Phase.tr = _ph_tr
Phase.act = _ph_act
Phase.dma = _ph_dma
Phase.v = _ph_v


def ph_prep(self):
    nc, K, I, Dm = self.nc, self.K, self.I, self.Dm
    with ExitStack() as es:
        ph = Phase(K, "prep_a")
        l0 = K.lane()
        lw = [K.lane(), K.lane()]
        rc = Res()
        ph.dma("sp", self.ident_f[:], I["ident_f"], l0, writes=[rc], nowaw=True)
        ph.dma("sp", self.ident_b[:], I["ident_b"], l0, writes=[rc], nowaw=True)
        ph.v("pool", "memset", self.ones_div[:], 1.0 / 1024.0, writes=[rc], nowaw=True)
        ph.v("pool", "memset", self.ones_f[:], 1.0, writes=[rc], nowaw=True)
        rows = self.sb(es, "rows", [128, 3, 128], F32)
        ph.dma("sp", rows[0:8, 0, :], I["c"], l0, writes=[rc], nowaw=True)
        ph.dma("sp", rows[0:96, 1, :], I["mod_b"], l0, writes=[rc], nowaw=True)
        ph.dma("sp", rows[0:64, 2, :], I["norm_g"], l0, writes=[rc], nowaw=True)
        psT = self.ps(es, "psT")
        psM = self.ps(es, "psM")
        rT = Res()
        ph.tr(psT[:, 0:8], rows[0:8, 0, :], self.ident_f[0:8, 0:8], reads=[rc], writes=[rT], )
        ph.tr(psT[:, 128:224], rows[0:96, 1, :], self.ident_f[0:96, 0:96], reads=[rc], writes=[rT])
        ph.tr(psT[:, 256:320], rows[0:64, 2, :], self.ident_f[0:64, 0:64], reads=[rc], writes=[rT])
        small = self.sb(es, "small", [128, 256], F32)
        rs = Res()
        ph.act(small[:, 0:8], psT[:, 0:8], AF.Silu, reads=[rT], writes=[rs])
        ph.v("dve", "tensor_copy", small[:, 8:104], psT[:, 128:224], reads=[rT], writes=[rs], )
        ph.v("dve", "tensor_copy", small[:, 104:168], psT[:, 256:320], reads=[rT], writes=[rs])
        mwp = Pool2([self.sb(es, "mw%d" % i, [128, 8, 1536], F32) for i in range(2)])
        rM = Res()
        for l in range(2):
            for ng in range(4):
                mw, rmw = mwp.next()
                lane = lw[(l * 4 + ng) % 2]
                for c in range(8):
                    ph.dma("sp" if c % 2 == 0 else "act", mw[:, c, :],
                           I["mod_w"][l, c * 128:(c + 1) * 128, ng * 1536:(ng + 1) * 1536], lane,
                           writes=[rmw], nowaw=(c > 0))
                for j in range(12):
                    col = l * 48 + ng * 12 + j
                    for c in range(8):
                        ph.mm(psM[:, col:col + 1], mw[:, c, j * 128:(j + 1) * 128], small[:, c:c + 1],
                              c == 0, c == 7, reads=[rmw, rs], writes=[rM])
        mods = small[:, 168:264] if False else None
        modsT = self.sb(es, "modsT", [128, 96], F32)
        rm = Res()
        ph.v("dve", "tensor_tensor", modsT[:], psM[:, 0:96], small[:, 8:104], ALU.add, reads=[rM, rs], writes=[rm])
        rv = Res()
        for l in range(2):
            b = l * 48
            g = lambda i: small[:, 104 + l * 32 + i * 8: 104 + l * 32 + i * 8 + 8]
            m = lambda i: modsT[:, b + i * 8: b + i * 8 + 8]
            V = self.vecs
            ph.v("dve", "scalar_tensor_tensor", V[:, l, 0, :], m(1), 1.0, g(0), ALU.add, ALU.mult, reads=[rm, rs], writes=[rv], )
            ph.v("dve", "tensor_copy", V[:, l, 1, :], m(0), reads=[rm], writes=[rv])
            ph.v("dve", "tensor_tensor", V[:, l, 2, :], m(2), g(1), ALU.mult, reads=[rm, rs], writes=[rv])
            ph.v("dve", "scalar_tensor_tensor", V[:, l, 3, :], m(4), 1.0, g(2), ALU.add, ALU.mult, reads=[rm, rs], writes=[rv])
            ph.v("dve", "tensor_copy", V[:, l, 4, :], m(3), reads=[rm], writes=[rv])
            ph.v("dve", "tensor_tensor", V[:, l, 5, :], m(5), g(3), ALU.mult, reads=[rm, rs], writes=[rv])
        if self.debug:
            dbgv = self.dscr("dbg_vecs", [128, 96], F32, dbg=True)
            ph.dma("sp", dbgv, self.vecs[:].rearrange("p a b c -> p (a b c)"), l0, reads=[rv])
        ph.emit()
    with ExitStack() as es:
        ph = Phase(K, "prep_b")
        xp = Pool2([self.sb(es, "xt%d" % i, [128, D], F32) for i in range(3)])
        xl = [K.lane() for _ in range(3)]
        hp = Pool2([self.sb(es, "hts%d" % i, [128, 8, 512], F32) for i in range(2)])
        hl = [K.lane() for _ in range(2)]
        banks = [self.ps(es, "pb%d" % i) for i in range(8)]
        rb = [Res() for _ in range(8)]
        hv = Dm["hT0"].rearrange("(c p) t -> p c t", p=128)
        k = 0
        pend_store = []
        for t in range(NT):
            xs = []
            for sub in range(4):
                xt, rx = xp.next()
                r0 = t * 512 + sub * 128
                ph.dma("sp", xt[:], I["x"][r0:r0 + 128, :], xl[k % 3], writes=[rx])
                k += 1
                for c in range(8):
                    ph.tr(banks[c][:, sub * 128:(sub + 1) * 128], xt[:, c * 128:(c + 1) * 128], self.ident_f[:],
                          reads=[rx], writes=[rb[c]])
            hts, rh = hp.next()
            for c in range(8):
                if c % 2 == 0:
                    ph.act(hts[:, c, :], banks[c][:], AF.Copy, reads=[rb[c]], writes=[rh])
                else:
                    ph.v("dve", "tensor_copy", hts[:, c, :], banks[c][:], reads=[rb[c]], writes=[rh])
            pend_store.append((t, hts, rh))
            if len(pend_store) > 1:
                t_, hts_, rh_ = pend_store.pop(0)
                ph.dma("sp", hv[:, :, t_ * 512:(t_ + 1) * 512], hts_[:], hl[t_ % 2], reads=[rh_])
        for t_, hts_, rh_ in pend_store:
            ph.dma("sp", hv[:, :, t_ * 512:(t_ + 1) * 512], hts_[:], hl[t_ % 2], reads=[rh_])
        ph.emit()
    with ExitStack() as es:
        ph = Phase(K, "prep_c")
        l0 = K.lane()
        tab = self.sb(es, "tab", [33, 17], F32)
        rt = Res()
        ph.v("dve", "memset", tab[:], 0.0, writes=[rt])
        ph.v("dve", "memset", tab[32:33, :], -10000.0, writes=[rt])
        ph.dma("sp", tab[0:32, 0:16], I["rbt"], l0, writes=[rt])
        ohc = self.sb(es, "ohc", [33, LC], F32)
        ohd = self.sb(es, "ohd", [33, 3, LD + 1], F32)
        ro = Res()
        ph.dma("sp", ohc[:], I["oh_c"], l0, writes=[ro], nowaw=True)
        ph.dma("sp", ohd[:], I["oh_d"].rearrange("g b m -> b g m"), l0, writes=[ro], nowaw=True)
        lbp = Pool2([self.sb(es, "lb%d" % i, [33, 128], F32) for i in range(2)])
        gp = Pool2([self.sb(es, "gsb%d" % i, [128, LC], BF16) for i in range(2)])
        gl = [K.lane(), K.lane()]
        gdp = Pool2([self.sb(es, "gdb%d" % i, [128, 3, LD + 1], BF16) for i in range(2)])
        gdl = [K.lane(), K.lane()]
        pg = Pool2([self.ps(es, "pg%d" % i) for i in range(4)])
        for s in range(9):
            slot = 8 + s
            lb, rl = lbp.next()
            ph.v("dve", "tensor_copy", lb[:], tab[:, slot:slot + 1].to_broadcast([33, 128]), reads=[rt], writes=[rl])
            gsb, rg = gp.next()
            for n0 in range(0, LC, 512):
                w = min(512, LC - n0)
                pb, rp = pg.next()
                ph.mm(pb[:, 0:w], lb[:], ohc[:, n0:n0 + w], True, True, reads=[rl, ro], writes=[rp])
                ph.act(gsb[:, n0:n0 + w], pb[:, 0:w], AF.Exp, reads=[rp], writes=[rg], )
            ph.dma("sp", Dm["gc"][s], gsb[:], gl[s % 2], reads=[rg])
        for h in range(16):
            lb, rl = lbp.next()
            ph.v("dve", "tensor_copy", lb[:], tab[:, h:h + 1].to_broadcast([33, 128]), reads=[rt], writes=[rl])
            gdb, rg = gdp.next()
            for g in range(3):
                pb, rp = pg.next()
                ph.mm(pb[:, 0:LD + 1], lb[:], ohd[:, g, :], True, True, reads=[rl, ro], writes=[rp])
                ph.act(gdb[:, g, :], pb[:, 0:LD + 1], AF.Exp, reads=[rp], writes=[rg])
            ph.dma("sp", Dm["gd"][h].rearrange("g p m -> p g m"), gdb[:], gdl[h % 2], reads=[rg])
        ph.emit()


Prog.ph_prep = ph_prep


def norm_stats(self, ph, src, rsrc, wk, width):
    sq, rsq = wk["sq"].next()
    ph.act(sq[:, :, 0:width], src, AF.Square, reads=[rsrc], writes=[rsq])
    pb, rp = wk["psn"].next()
    for c in range(8):
        ph.mm(pb[:, 0:width], self.ones_div[:], sq[:, c, 0:width], c == 0, c == 7, reads=[rsq], writes=[rp])
    sd, rsd = wk["sd"].next()
    ph.act(sd[:, 0:width], pb[:, 0:width], AF.Sqrt, reads=[rp, wk["reps"]], writes=[rsd], bias=wk["eps"][:, 0:1])
    rstd, rr = wk["rstd"].next()
    ph.v("dve", "reciprocal", rstd[:, 0:width], sd[:, 0:width], reads=[rsd], writes=[rr])
    return rstd, rr


def norm_modulate(self, ph, ht, rh, layer, which, out, rout, wk, width=512):
    rstd, rr = self.norm_stats(ph, ht, rh, wk, width)
    tt, rt = wk["tt"].next()
    ph.v("dve", "tensor_tensor", tt[:, :, 0:width], ht, rstd[:, None, 0:width].to_broadcast([128, 8, width]), ALU.mult,
         reads=[rh, rr], writes=[rt])
    gi = 0 if which == 1 else 3
    for c in range(8):
        gs = self.vecs[:, layer, gi, c:c + 1]
        sh = self.vecs[:, layer, gi + 1, c:c + 1]
        if c % 2 == 0:
            ph.add("act", lambda e, c=c, gs=gs, sh=sh: e.activation(out=out[:, c, :], in_=tt[:, c, 0:width], func=AF.Identity,
                                                                    bias=sh, scale=gs), [rt], [rout], nowaw=True)
        else:
            ph.add("pool", lambda e, c=c, gs=gs, sh=sh: e.tensor_scalar(out[:, c, :], tt[:, c, 0:width], gs, sh, ALU.mult, ALU.add),
                   [rt], [rout], nowaw=True)


def post_norm_residual(self, ph, yT, ry, ht, rh, layer, which, out, rout, wk, width=512):
    rstd, rr = self.norm_stats(ph, yT, ry, wk, width)
    tt, rt = wk["tt"].next()
    ph.v("dve", "tensor_tensor", tt[:, :, 0:width], yT, rstd[:, None, 0:width].to_broadcast([128, 8, width]), ALU.mult,
         reads=[ry, rr], writes=[rt])
    gi = 2 if which == 1 else 5
    for c in range(8):
        gg = self.vecs[:, layer, gi, c:c + 1]
        eng = "dve"
        ph.add(eng, lambda e, c=c, gg=gg: e.scalar_tensor_tensor(out[:, c, :], tt[:, c, 0:width], gg, ht[:, c, :], ALU.mult, ALU.add),
               [rt, rh], [rout], nowaw=True)


def make_wk(self, es, ph, width=512, nbuf=1):
    wk = {}
    wk["sq"] = Pool2([self.sb(es, "wk_sq%d" % i, [128, 8, width], BF16) for i in range(nbuf)])
    wk["tt"] = Pool2([self.sb(es, "wk_tt%d" % i, [128, 8, width], F32) for i in range(nbuf)])
    wk["sd"] = Pool2([self.sb(es, "wk_sd%d" % i, [128, width], F32) for i in range(2)])
    wk["rstd"] = Pool2([self.sb(es, "wk_rs%d" % i, [128, width], F32) for i in range(2)])
    wk["psn"] = Pool2([self.ps(es, "wk_ps%d" % i) for i in range(2)])
    wk["eps"] = self.sb(es, "wk_eps", [128, 1], F32)
    wk["reps"] = Res()
    ph.v("pool", "memset", wk["eps"][:], EPS, writes=[wk["reps"]])
    return wk


def load_weight_bf16(self, ph, es, name, src, K_rows, N, lane, colstep=1024):
    kc = K_rows // 128
    W = self.sb(es, name, [128, kc, N], BF16)
    rW = Res()
    first = True
    for c in range(kc):
        for n0 in range(0, N, colstep):
            w = min(colstep, N - n0)
            ph.dma("pool", W[:, c, n0:n0 + w], src[c * 128:(c + 1) * 128, n0:n0 + w], lane, writes=[rW], nowaw=not first)
            first = False
    return W, rW


Prog.norm_stats = norm_stats
Prog.norm_modulate = norm_modulate
Prog.post_norm_residual = post_norm_residual
Prog.make_wk = make_wk
Prog.load_weight_bf16 = load_weight_bf16


def ph_inproj(self, layer):
    nc, K, I, Dm = self.nc, self.K, self.I, self.Dm
    hsrc = Dm["hT0"] if layer == 0 else Dm["hT2"]
    hv = hsrc.rearrange("(c p) t -> p c t", p=128)
    with ExitStack() as es:
        ph = Phase(K, "inproj%d" % layer)
        wl = K.lane()
        NW = 3080 if layer == 0 else 3072
        W, rW = self.load_weight_bf16(ph, es, "Win", I["in_w0"] if layer == 0 else I["in_w1"], D, NW, wl)
        wk = self.make_wk(es, ph)
        uT = self.sb(es, "uTall", [128, 8, S], BF16)
        ru = [Res() for _ in range(NT)]
        htp = Pool2([self.sb(es, "ht0", [128, 8, 512], F32)])
        hl = K.lane()
        for t in range(NT):
            ht, rh = htp.next()
            ph.dma("sp", ht[:], hv[:, :, t * 512:(t + 1) * 512], hl, writes=[rh])
            self.norm_modulate(ph, ht[:], rh, layer, 1, uT[:, :, t * 512:(t + 1) * 512], ru[t], wk)
        if self.debug:
            dbu = self.dscr("dbg_uT%d" % layer, [D, S], BF16, dbg=True)
            ph.dma("sp", dbu.rearrange("(c p) t -> p c t", p=128), uT[:], hl, reads=ru)
        if layer == 0:
            qk_cols = [i * 128 for i in range(8)] + [1544 + i * 128 for i in range(8)]
            is_q = [True] * 4 + [False] * 4 + [True] * 4 + [False] * 4
            v_cols = [1024, 2568]
        else:
            qk_cols = [i * 128 for i in range(16)]
            is_q = [True] * 8 + [False] * 8
            v_cols = [2048, 2560]
        pq = Pool2([self.ps(es, "pq%d" % i) for i in range(3)])
        qsb = Pool2([self.sb(es, "qsb%d" % i, [128, 512], BF16) for i in range(3)])
        ql = [K.lane() for _ in range(3)]
        k = 0
        for t in range(NT):
            for oc in range(16):
                pb, rp = pq.next()
                for c in range(8):
                    ph.mm(pb[:], W[:, c, qk_cols[oc]:qk_cols[oc] + 128], uT[:, c, t * 512:(t + 1) * 512], c == 0, c == 7,
                          reads=[rW, ru[t]], writes=[rp])
                qs, rq = qsb.next()
                sc = 0.125 if is_q[oc] else 1.0
                if k % 2 == 0:
                    ph.act(qs[:], pb[:], AF.Copy, reads=[rp], writes=[rq], scale=sc)
                else:
                    ph.v("dve", "tensor_scalar", qs[:], pb[:], sc, None, ALU.mult, reads=[rp], writes=[rq])
                ph.dma("sp", Dm["qkT"][oc * 128:(oc + 1) * 128, t * 512:(t + 1) * 512], qs[:], ql[k % 3], reads=[rq])
                k += 1
        if layer == 0:
            fg = self.sb(es, "fgb", [8, 2], F32)
            rfg = Res()
            ph.dma("sp", fg[:, 0:1], I["fgb"], hl, writes=[rfg])
            ph.v("dve", "tensor_scalar", fg[:, 1:2], fg[:, 0:1], -1.0, None, ALU.mult, reads=[rfg], writes=[rfg])
            pf = self.ps(es, "pf")
            rpf = Res()
            fsb = Pool2([self.sb(es, "fsb%d" % i, [8, 2, 512], F32) for i in range(2)])
            fl = [K.lane(), K.lane()]
            for t in range(NT):
                for c in range(8):
                    ph.mm(pf[0:8, :], W[:, c, 1536:1544], uT[:, c, t * 512:(t + 1) * 512], c == 0, c == 7,
                          reads=[rW, ru[t]], writes=[rpf])
                fs, rf = fsb.next()
                ph.act(fs[:, 0, :], pf[0:8, :], AF.Exp, reads=[rpf, rfg], writes=[rf], bias=fg[:, 1:2], scale=-1.0)
                ph.act(fs[:, 1, :], fs[:, 0, :], AF.Ln, reads=[rf], writes=[rf], bias=1.0)
                ph.dma("sp", self.spT[:, t * 512:(t + 1) * 512], fs[:, 1, :], fl[t % 2], reads=[rf])
        pv = Pool2([self.ps(es, "pv%d" % i) for i in range(2)])
        vtp = Pool2([self.sb(es, "vt%d" % i, [128, 16, 4, 65], BF16) for i in range(2)])
        vl = [K.lane(), K.lane()]
        for vt_, rv_ in zip(vtp.tiles, vtp.res):
            ph.v("pool", "memset", vt_[:], 1.0, writes=[rv_])
        pats = [1] if layer == 0 else list(DILS)
        k = 0
        for gi, dil in enumerate(pats):
            Ls = S // dil
            for grp in range(8):
                vt, rv = vtp.next()
                for b4 in range(4):
                    m0 = (grp * 4 + b4) * 128
                    r, n0 = m0 // Ls, m0 % Ls
                    t0 = n0 * dil + r
                    tiles_touched = sorted(set([(t0 + j * dil) // 512 for j in (0, 127)]))
                    tiles_touched = list(range(tiles_touched[0], tiles_touched[-1] + 1))
                    for half in range(2):
                        pb, rp = pv.next()
                        for c in range(8):
                            ph.mm(pb[:], uT[:, c, t0:t0 + 127 * dil + 1:dil], W[:, c, v_cols[half]:v_cols[half] + 512],
                                  c == 0, c == 7, reads=[rW] + [ru[x] for x in tiles_touched], writes=[rp])
                        src = pb[:].rearrange("p (h e) -> p h e", e=64)
                        dst = vt[:, half * 8:(half + 1) * 8, b4, 0:64]
                        if k % 2 == 0:
                            ph.act(dst, src, AF.Copy, reads=[rp], writes=[rv], )
                        else:
                            ph.v("dve", "tensor_copy", dst, src, reads=[rp], writes=[rv])
                        k += 1
                dv = Dm["vaug"][gi][:, :, grp * 260:(grp + 1) * 260].rearrange("h p f -> p h f")
                ph.dma("sp", dv, vt[:].rearrange("p h b e -> p h (b e)"), vl[grp % 2], reads=[rv])
        ph.emit()


Prog.ph_inproj = ph_inproj


def ph_cum(self):
    nc, K, I, Dm = self.nc, self.K, self.I, self.Dm
    CH = 1024
    with ExitStack() as es:
        ph = Phase(K, "cum")
        l0, l1 = K.lane(), K.lane()
        ones = self.sb(es, "c_ones", [8, CH], F32)
        r1 = Res()
        ph.v("dve", "memset", ones[:], 1.0, writes=[r1])
        spp = Pool2([self.sb(es, "c_sp%d" % i, [8, CH], F32) for i in range(2)])
        Sp = Pool2([self.sb(es, "c_S%d" % i, [8, CH], F32) for i in range(2)])
        cpp = Pool2([self.sb(es, "c_cp%d" % i, [8, 6, CH], BF16) for i in range(2)])
        ra = self.sb(es, "c_ra", [8, CH], F32)
        rb = self.sb(es, "c_rb", [8, CH], F32)
        rr = Res()
        prevS = None
        for ci in range(S // CH):
            sp, rs = spp.next()
            ph.dma("sp", sp[:], self.spT[:, ci * CH:(ci + 1) * CH], l0, writes=[rs])
            Sc, rS = Sp.next()
            init = 0.0 if prevS is None else prevS[0][:, CH - 1:CH]
            rd = [rs, r1] + ([] if prevS is None else [prevS[1]])
            ph.v("dve", "tensor_tensor_scan", Sc[:], ones[:], sp[:], init, ALU.mult, ALU.add, reads=rd, writes=[rS])
            prevS = (Sc, rS)
            cp, rc = cpp.next()
            ph.v("dve", "tensor_copy", cp[:, 0, :], Sc[:], reads=[rS], writes=[rc])
            ph.v("dve", "tensor_tensor", ra[:], Sc[:], cp[:, 0, :], ALU.subtract, reads=[rS, rc], writes=[rr])
            ph.v("dve", "tensor_copy", cp[:, 1, :], ra[:], reads=[rr], writes=[rc])
            ph.v("dve", "tensor_tensor", rb[:], ra[:], cp[:, 1, :], ALU.subtract, reads=[rr, rc], writes=[rr])
            ph.v("dve", "tensor_copy", cp[:, 2, :], rb[:], reads=[rr], writes=[rc])
            ph.v("dve", "tensor_scalar", cp[:, 3:6, :], cp[:, 0:3, :], -1.0, None, ALU.mult, reads=[rc], writes=[rc])
            ph.dma("sp", Dm["cparts"][:, :, ci * CH:(ci + 1) * CH], cp[:], l1, reads=[rc])
        ph.emit()


Prog.ph_cum = ph_cum


def _in_maps(inputs, cores):
    c = make_consts()
    f = lambda a: np.ascontiguousarray(np.asarray(a, dtype=np.float32))
    shared = {
        "mod_w": f(inputs["mod_w"]),
        "mod_b": f(inputs["mod_b"]).reshape(96, 128),
        "norm_g": f(inputs["norm_g"]).reshape(64, 128),
        "in_w0": f(inputs["attn_in_w_even"][0]),
        "fgb": f(inputs["fox_gate_bias"][0]).reshape(8, 1),
        "out_w0": f(inputs["attn_out_w_even"][0]),
        "in_w1": f(inputs["attn_in_w_odd"][0]),
        "out_w1": f(inputs["attn_out_w_odd"][0]),
        "rbt": f(inputs["rel_bias_table"]),
        "ffn_g": f(inputs["ffn_w_gate"]),
        "ffn_u": f(inputs["ffn_w_up"]),
        "ffn_d": f(inputs["ffn_w_down"]),
        "router": f(inputs["router_w"][0]),
        "exp_g": f(inputs["exp_w_gate"][0]),
        "exp_u": f(inputs["exp_w_up"][0]),
        "exp_d": f(inputs["exp_w_down"][0]),
    }
    shared.update(c)
    maps = []
    for b in cores:
        m = dict(shared)
        m["x"] = f(inputs["x"][b])
        m["c"] = f(inputs["c"][b]).reshape(8, 128)
        maps.append(m)
    return maps


def kernel(**inputs):
    prog = Prog()
    nc = prog.build()
    maps = _in_maps(inputs, list(range(8)))
    res = run_bass_kernel_spmd(nc, maps, core_ids=list(range(8)))
    return np.stack([np.asarray(r["out"], dtype=np.float32) for r in res.results], axis=0)


def ph_attn01(self, moba):
    nc, K, I, Dm = self.nc, self.K, self.I, self.Dm
    KD = 80 if moba else 70
    gc = Dm["gc"]
    with ExitStack() as es:
        ph = Phase(K, "moba" if moba else "fox")
        Qa = [self.sb(es, "Qa%d" % i, [128, S], BF16) for i in range(2)]
        Ka = [self.sb(es, "Ka%d" % i, [128, S], BF16) for i in range(2)]
        Vg = [self.sb(es, "Vg%d" % i, [128, 32 * 65 + 64], BF16) for i in range(2)]
        rQ = [Res(), Res()]
        rK = [Res(), Res()]
        rV = [Res(), Res()]
        for b in range(2):
            ph.v("pool", "memset", Qa[b][64:128, :], 0.0, writes=[rQ[b]])
            ph.v("pool", "memset", Ka[b][64:128, :], 0.0, writes=[rK[b]])
            ph.v("pool", "memset", Vg[b][:, 2080:2144], 0.0, writes=[rV[b]])
        lq = [K.lane(), K.lane()]
        lk = [K.lane(), K.lane()]
        lv = [K.lane(), K.lane()]
        lo = [K.lane(), K.lane()]
        Oacc = [self.sb(es, "Oacc%d" % i, [128, S], F32) for i in range(2)]
        rOa = [Res(), Res()]
        ptp = Pool2([self.sb(es, "Pt%d" % i, [128, 512], BF16) for i in range(8)])
        psS = Pool2([self.ps(es, "psS%d" % i) for i in range(3 if moba else 4)])
        psO = Pool2([self.ps(es, "psO%d" % i) for i in range(2)])
        psL = Pool2([self.ps(es, "psL%d" % i) for i in range(1)])
        rinvp = Pool2([self.sb(es, "rinv%d" % i, [128, 512], F32) for i in range(2)])
        osbp = Pool2([self.sb(es, "osb%d" % i, [128, 512], BF16) for i in range(2)])
        if moba:
            Th = [self.sb(es, "Th%d" % i, [128, WC], BF16) for i in range(2)]
            rT = [Res(), Res()]
            lt = [K.lane(), K.lane()]
            for b in range(2):
                ph.dma("sp", Ka[b][64:80, :], I["ind"], lk[b], writes=[rK[b]])
            psG = self.ps(es, "psG")
            rG = Res()
            psT = self.ps(es, "psT")
            rPT = Res()
            ksf = self.sb(es, "ksf", [128, 16], F32)
            ksb = self.sb(es, "ksb", [128, 16], BF16)
            rks = Res()
            gsb = self.sb(es, "gsb", [128, 16], F32)
            mx8 = self.sb(es, "mx8", [128, 8], F32)
            selT = self.sb(es, "selT", [128, 80], BF16)
            rg = Res()
            rsel = Res()
            ph.v("dve", "memset", selT[:], 0.0, writes=[rsel])
        else:
            T0 = self.sb(es, "T0", [128, WC], BF16)
            rT0 = Res()
            lt0 = K.lane()
            ph.dma("sp", T0[:], AP(gc.tensor, 8 * 128 * LC + 127, [[LC - 1, 128], [1, WC]]), lt0, writes=[rT0])
            tmpp = Pool2([self.sb(es, "tmp%d" % i, [128, 512], F32) for i in range(2)])
            for b in range(2):
                ph.v("pool", "memset", Qa[b][64:70, :], 1.0, writes=[rQ[b]])
                ph.v("pool", "memset", Ka[b][64:70, :], 1.0, writes=[rK[b]])
        kk = 0
        LA = 5

        def prep_loads(h):
            b = h % 2
            qrow = (1024 if moba else 0) + h * 64
            krow = (1536 if moba else 512) + h * 64
            hg = (8 + h) if moba else h
            ph.dma("sp", Qa[b][0:64, :], Dm["qkT"][qrow:qrow + 64, :], lq[b], writes=[rQ[b]])
            ph.dma("sp", Ka[b][0:64, :], Dm["qkT"][krow:krow + 64, :], lk[b], writes=[rK[b]])
            ph.dma("sp", Vg[b][:, 0:2080], Dm["vaug"][0][hg], lv[b], writes=[rV[b]])
            if moba:
                ph.dma("sp", Th[b][:], AP(gc.tensor, h * 128 * LC + 127, [[LC - 1, 128], [1, WC]]), lt[b], writes=[rT[b]])
            else:
                ph.dma("sp", Qa[b][64:67, :], Dm["cparts"][h, 3:6, :], lq[b], writes=[rQ[b]], nowaw=True)
                ph.dma("sp", Ka[b][67:70, :], Dm["cparts"][h, 0:3, :], lk[b], writes=[rK[b]], nowaw=True)

        def gate_chunks(h):
            b = h % 2
            ch = []
            if not moba:
                return ch

            def c0():
                ph.v("dve", "tensor_reduce", ksf[0:64, :], Ka[b][0:64, :].rearrange("p (j s) -> p j s", s=256), AX.X, ALU.add,
                     reads=[rK[b]], writes=[rks])
                ph.v("dve", "tensor_copy", ksb[0:64, :], ksf[0:64, :], reads=[rks], writes=[rks])
                ph.v("dve", "memset", gsb[:], -1e30, writes=[rg])
                ph.v("dve", "memset", selT[:, 64:80], NEG, writes=[rsel])
            ch.append(c0)
            for tt in range(32):
                def cA(tt=tt):
                    own = tt // 2
                    if tt % 2 == 0:
                        ph.v("dve", "memset", selT[:, 64 + own:65 + own], 0.0, writes=[rsel])
                    if own >= 3:
                        ph.mm(psG[:, 0:16], Qa[b][0:64, tt * 128:(tt + 1) * 128], ksb[0:64, 0:16], True, True,
                              reads=[rQ[b], rks], writes=[rG])
                        ph.v("dve", "tensor_copy", gsb[:, 0:own], psG[:, 0:own], reads=[rG], writes=[rg])
                        ph.v("dve", "max", mx8[:], gsb[:, 0:16], reads=[rg], writes=[rg])
                        ph.v("dve", "tensor_scalar", selT[:, 64:64 + own], gsb[:, 0:own], mx8[:, 2:3], NEG, ALU.is_lt, ALU.mult,
                             reads=[rg], writes=[rsel])

                def cC(tt=tt):
                    ph.mm(psT[0:80, 0:128], selT[:, 0:80], self.ident_b[:], True, True, reads=[rsel], writes=[rPT])
                    ph.add("act", lambda e, tt=tt: e.activation(out=Qa[b][64:80, tt * 128:(tt + 1) * 128], in_=psT[64:80, 0:128],
                                                                func=AF.Copy), [rPT], [rQ[b]], nowaw=True)
                ch.append(cA)
                ch.append(cC)
            return ch

        def sweep(h, chunks):
            nonlocal kk
            b = h % 2
            hg = (8 + h) if moba else h
            items = [(qt, kb) for qt in range(NT) for kb in range(4 * (qt + 1))]
            st = {}

            def stage1(it):
                nonlocal kk
                qt, kb = it
                pS, rS = psS.next()
                ph.mm(pS[:], Ka[b][:, kb * 128:(kb + 1) * 128], Qa[b][:, qt * 512:(qt + 1) * 512], True, True,
                      reads=[rK[b], rQ[b]], writes=[rS])
                Pt, rP = ptp.next()
                delta = 512 * qt - 128 * kb
                if moba:
                    ph.act(Pt[:], pS[:], AF.Exp, reads=[rS], writes=[rP])
                    a = min(delta, 2304) + 384
                    ph.v("pool" if kk % 4 == 3 else "dve", "tensor_tensor", Pt[:], Pt[:], Th[b][:, a:a + 512], ALU.mult,
                         reads=[rP, rT[b]], writes=[rP])
                    kk += 1
                elif delta <= 0:
                    tmp, rtm = tmpp.next()
                    ph.v("dve", "tensor_scalar", tmp[:], pS[:], 60.0, None, ALU.min, reads=[rS], writes=[rtm])
                    ph.act(Pt[:], tmp[:], AF.Exp, reads=[rtm], writes=[rP])
                    a = delta + 384
                    ph.v("dve", "tensor_tensor", Pt[:], Pt[:], T0[:, a:a + 512], ALU.mult, reads=[rP, rT0], writes=[rP])
                else:
                    ph.act(Pt[:], pS[:], AF.Exp, reads=[rS], writes=[rP])
                st[it] = (Pt, rP)

            def stage2(it):
                qt, kb = it
                nkb = 4 * (qt + 1)
                if kb == 0:
                    st["O"] = psO.next()
                pO, rO = st["O"]
                Pt, rP = st.pop(it)
                ph.mm(pO[:, :], Vg[b][:, kb * 65:kb * 65 + 128], Pt[:], kb == 0, kb == nkb - 1, reads=[rV[b], rP], writes=[rO])
                if kb == nkb - 1:
                    self.finish_chunk(ph, pO, rO, Oacc[b], rOa[b], qt, hg, psL, rinvp, osbp, lo, first=(qt == 0))

            for i in range(len(items) + LA):
                if i < len(items):
                    stage1(items[i])
                if i >= LA:
                    stage2(items[i - LA])
                if chunks and i % 2 == 1:
                    chunks.pop(0)()
            while chunks:
                chunks.pop(0)()

        prep_loads(0)
        for c_ in gate_chunks(0):
            c_()
        for h in range(8):
            nxt = []
            if h + 1 < 8:
                prep_loads(h + 1)
                nxt = gate_chunks(h + 1)
            sweep(h, nxt)
        ph.emit()


def finish_chunk(self, ph, pO, rO, Oacc, rOa, qt, hg, psL, rinvp, osbp, lo, first, src_is_sbuf=False):
    cs = slice(qt * 512, (qt + 1) * 512)
    if not src_is_sbuf:
        ph.add("act", lambda e: e.activation(out=Oacc[0:65, cs], in_=pO[0:65, :], func=AF.Copy), [rO], [rOa], nowaw=not first)
    pL, rL = psL.next()
    ph.mm(pL[0:64, :], self.ones_f[64:65, 0:64], Oacc[64:65, cs], True, True, reads=[rOa], writes=[rL])
    rinv, rri = rinvp.next()
    ph.act(rinv[0:64, :], pL[0:64, :], AF.Ln, reads=[rL], writes=[rri])
    ph.act(rinv[0:64, :], rinv[0:64, :], AF.Exp, reads=[rri], writes=[rri], scale=-1.0)
    osb, ros = osbp.next()
    ph.v("pool", "tensor_tensor", osb[0:64, :], Oacc[0:64, cs], rinv[0:64, :], ALU.mult, reads=[rOa, rri], writes=[ros])
    ph.dma("sp", self.Dm["oT"][hg * 64:(hg + 1) * 64, cs], osb[0:64, :], lo[qt % 2], reads=[ros])


Prog.ph_attn01 = ph_attn01
Prog.finish_chunk = finish_chunk


def ph_outproj(self, layer):
    nc, K, I, Dm = self.nc, self.K, self.I, self.Dm
    hsrc = Dm["hT0"] if layer == 0 else Dm["hT2"]
    hdst = Dm["hT1"] if layer == 0 else Dm["hT3"]
    hv = hsrc.rearrange("(c p) t -> p c t", p=128)
    hd = hdst.rearrange("(c p) t -> p c t", p=128)
    ov = Dm["oT"].rearrange("(c p) t -> p c t", p=128)
    with ExitStack() as es:
        ph = Phase(K, "outproj%d" % layer)
        Wo, rW = self.load_weight_bf16(ph, es, "Wo", I["out_w0"] if layer == 0 else I["out_w1"], D, D, K.lane())
        wk = self.make_wk(es, ph)
        otp = Pool2([self.sb(es, "ot%d" % i, [128, 8, 512], BF16) for i in range(2)])
        htp = Pool2([self.sb(es, "ht%d" % i, [128, 8, 512], F32) for i in range(2)])
        ytp = Pool2([self.sb(es, "yt%d" % i, [128, 8, 512], F32) for i in range(2)])
        l1 = [K.lane(), K.lane()]
        l2 = [K.lane(), K.lane()]
        l3 = [K.lane(), K.lane()]
        py = Pool2([self.ps(es, "py%d" % i) for i in range(4)])
        def loads(t):
            ts_ = slice(t * 512, (t + 1) * 512)
            ot, ro = otp.next()
            ph.dma("sp", ot[:], ov[:, :, ts_], l1[t % 2], writes=[ro])
            ht, rh = htp.next()
            ph.dma("sp", ht[:], hv[:, :, ts_], l2[t % 2], writes=[rh])
            return ot, ro, ht, rh

        nxt = loads(0)
        for t in range(NT):
            ts_ = slice(t * 512, (t + 1) * 512)
            ot, ro, ht, rh = nxt
            if t + 1 < NT:
                nxt = loads(t + 1)
            yt, ry = ytp.next()
            for dc in range(8):
                pb, rp = py.next()
                for c in range(8):
                    ph.mm(pb[:], Wo[:, c, dc * 128:(dc + 1) * 128], ot[:, c, :], c == 0, c == 7, reads=[rW, ro], writes=[rp])
                if dc % 2 == 0:
                    ph.add("act", lambda e, yt=yt, pb=pb, dc=dc: e.activation(out=yt[:, dc, :], in_=pb[:], func=AF.Copy), [rp], [ry],
                           nowaw=(dc > 0))
                else:
                    ph.add("dve", lambda e, yt=yt, pb=pb, dc=dc: e.tensor_copy(yt[:, dc, :], pb[:]), [rp], [ry], nowaw=True)
            self.post_norm_residual(ph, yt[:], ry, ht[:], rh, layer, 1, ht[:], rh, wk)
            ph.dma("sp", hd[:, :, ts_], ht[:], l3[t % 2], reads=[rh])
        ph.emit()


Prog.ph_outproj = ph_outproj


def ph_ffn(self, layer):
    nc, K, I, Dm = self.nc, self.K, self.I, self.Dm
    E = 1 if layer == 0 else 8
    F = 2816 if layer == 0 else 3584
    nf = F // 128
    groups = []
    f0 = 0
    while f0 < nf:
        n = min(4, nf - f0)
        groups.append((f0, n))
        f0 += n
    hsrc = Dm["hT1"] if layer == 0 else Dm["hT3"]
    hv = hsrc.rearrange("(c p) t -> p c t", p=128)
    hd = Dm["hT2"].rearrange("(c p) t -> p c t", p=128)
    if layer == 0:
        Wg, Wu, Wd = I["ffn_g"], I["ffn_u"], I["ffn_d"]
    else:
        Wg, Wu, Wd = I["exp_g"], I["exp_u"], I["exp_d"]
    HALF = 2048
    SW = 256
    with ExitStack() as es:
        ph = Phase(K, "ffn%d" % layer)
        wk = self.make_wk(es, ph, width=SW)
        yacc = self.sb(es, "yacc", [128, 8, HALF], F32)
        uT = self.sb(es, "uTh", [128, 8, HALF], BF16)
        wgp = [self.sb(es, "wg%d" % i, [128, 8, 512], BF16) for i in range(2)]
        wup = [self.sb(es, "wu%d" % i, [128, 8, 512], BF16) for i in range(2)]
        wdp = [self.sb(es, "wd%d" % i, [128, 4, D], BF16) for i in range(2)]
        rw = [Res(), Res()]
        lw = [K.lane(), K.lane()]
        hmid = self.sb(es, "hmid", [128, 4, 512], BF16)
        rhm = Res()
        sp_ = Pool2([self.sb(es, "ssb%d" % i, [128, 512], BF16) for i in range(2)])
        htp = Pool2([self.sb(es, "hts", [128, 8, SW], F32)])
        lh = K.lane()
        lst = K.lane()
        pa = Pool2([self.ps(es, "pa%d" % i) for i in range(2)])
        pbk = Pool2([self.ps(es, "pb%d" % i) for i in range(2)])
        pyk = Pool2([self.ps(es, "py%d" % i) for i in range(2)])
        if E > 1:
            bgp = Pool2([self.sb(es, "bg%d" % i, [128, 512], F32) for i in range(2)])
            gbc = self.sb(es, "gbc", [128, HALF], F32)
            rgb = Res()
            gT = self.sb(es, "gT", [8, HALF], F32)
            rgT = Res()
            selE = self.sb(es, "selE", [8, 8, 128], F32)
            rse = Res()
            ph.v("dve", "tensor_copy", selE[:], self.ident_f[0:8, 0:8, None].to_broadcast([8, 8, 128]), writes=[rse])
            Wr, rWr = self.load_weight_bf16(ph, es, "Wr", I["router"], D, 8, K.lane())
            lg = self.sb(es, "lg", [128, 8], F32)
            mx = self.sb(es, "mx", [128, 8], F32)
            pr = self.sb(es, "pr", [128, 4], F32)
            gg = self.sb(es, "gg", [128, 2, 8], F32)
            rl = Res()
            otp = Pool2([self.sb(es, "otile%d" % i, [128, D], F32) for i in range(1)])
            lot = [K.lane(), K.lane()]
        wcount = 0
        for hf in range(S // HALF):
            ru = Res()
            for sub in range(HALF // SW):
                tok = hf * HALF + sub * SW
                ht, rh = htp.next()
                ph.dma("sp", ht[:], hv[:, :, tok:tok + SW], lh, writes=[rh])
                self.norm_modulate(ph, ht[:], rh, layer, 2, uT[:, :, sub * SW:(sub + 1) * SW], ru, wk, width=SW)
                if E > 1:
                    for s2 in range(SW // 128):
                        c0 = sub * SW + s2 * 128
                        pb, rp = pbk.next()
                        for c in range(8):
                            ph.mm(pb[:, 0:8], uT[:, c, c0:c0 + 128], Wr[:, c, 0:8], c == 0, c == 7, reads=[ru, rWr], writes=[rp])
                        ph.v("dve", "tensor_copy", lg[:], pb[:, 0:8], reads=[rp], writes=[rl])
                        ph.v("dve", "max", mx[:], lg[:], reads=[rl], writes=[rl])
                        ph.v("dve", "tensor_tensor", pr[:, 0:1], mx[:, 0:1], mx[:, 1:2], ALU.subtract, reads=[rl], writes=[rl])
                        ph.act(pr[:, 1:2], pr[:, 0:1], AF.Sigmoid, reads=[rl], writes=[rl])
                        ph.act(pr[:, 2:3], pr[:, 0:1], AF.Sigmoid, reads=[rl], writes=[rl], scale=-1.0)
                        ph.v("dve", "tensor_scalar", gg[:, 0, :], lg[:], mx[:, 0:1], pr[:, 1:2], ALU.is_ge, ALU.mult, reads=[rl], writes=[rl])
                        ph.v("dve", "tensor_scalar", gg[:, 1, :], lg[:], mx[:, 1:2], pr[:, 2:3], ALU.is_equal, ALU.mult, reads=[rl], writes=[rl])
                        ph.v("dve", "tensor_tensor", gg[:, 0, :], gg[:, 0, :], gg[:, 1, :], ALU.add, reads=[rl], writes=[rl])
                        pb2, rp2 = pbk.next()
                        ph.tr(pb2[0:8, 0:128], gg[:, 0, :], self.ident_f[:], reads=[rl], writes=[rp2])
                        ph.add("act", lambda e, c0=c0, pb2=pb2: e.activation(out=gT[0:8, c0:c0 + 128], in_=pb2[0:8, 0:128], func=AF.Copy),
                               [rp2], [rgT], nowaw=True)
            for e_ in range(E):
                if E > 1:
                    for q in range(HALF // 512):
                        pb, rp = pbk.next()
                        ph.mm(pb[:], selE[0:8, e_, :], gT[0:8, q * 512:(q + 1) * 512], True, True, reads=[rse, rgT], writes=[rp])
                        ph.add("act", lambda e, q=q, pb=pb: e.activation(out=gbc[:, q * 512:(q + 1) * 512], in_=pb[:], func=AF.Copy),
                               [rp], [rgb], nowaw=(q > 0))
                for gi_, (f0, n) in enumerate(groups):
                    wb = wcount % 2
                    wcount += 1
                    wg, wu, wd = wgp[wb], wup[wb], wdp[wb]
                    first = True
                    for c in range(8):
                        ph.dma("pool", wg[:, c, 0:n * 128], Wg[e_, c * 128:(c + 1) * 128, f0 * 128:(f0 + n) * 128], lw[wb],
                               writes=[rw[wb]], nowaw=not first)
                        first = False
                        ph.dma("pool", wu[:, c, 0:n * 128], Wu[e_, c * 128:(c + 1) * 128, f0 * 128:(f0 + n) * 128], lw[wb],
                               writes=[rw[wb]], nowaw=True)
                    for j in range(n):
                        ph.dma("pool", wd[:, j, :], Wd[e_, (f0 + j) * 128:(f0 + j + 1) * 128, :], lw[wb], writes=[rw[wb]], nowaw=True)
                    for tq in range(HALF // 512):
                        tsl = slice(tq * 512, (tq + 1) * 512)
                        for j in range(n):
                            pA, rA = pa.next()
                            pB, rB = pbk.next()
                            for c in range(8):
                                ph.mm(pA[:], wg[:, c, j * 128:(j + 1) * 128], uT[:, c, tsl], c == 0, c == 7, reads=[rw[wb], ru], writes=[rA])
                            for c in range(8):
                                ph.mm(pB[:], wu[:, c, j * 128:(j + 1) * 128], uT[:, c, tsl], c == 0, c == 7, reads=[rw[wb], ru], writes=[rB])
                            ss, rs = sp_.next()
                            ph.act(ss[:], pA[:], AF.Silu, reads=[rA], writes=[rs])
                            if E > 1:
                                bg, rbg = bgp.next()
                                ph.v("dve", "tensor_tensor", bg[:], pB[:], gbc[:, tsl], ALU.mult, reads=[rB, rgb], writes=[rbg])
                                ph.add("pool", lambda e, j=j, ss=ss, bg=bg: e.tensor_tensor(hmid[:, j, :], ss[:], bg[:], ALU.mult),
                                       [rs, rbg], [rhm], nowaw=(j > 0))
                            else:
                                ph.add("dve", lambda e, j=j, ss=ss, pB=pB: e.tensor_tensor(hmid[:, j, :], ss[:], pB[:], ALU.mult),
                                       [rs, rB], [rhm], nowaw=(j > 0))
                        for dc in range(8):
                            pY, rY = pyk.next()
                            for j in range(n):
                                ph.mm(pY[:], wd[:, j, dc * 128:(dc + 1) * 128], hmid[:, j, :], j == 0, j == n - 1, reads=[rw[wb], rhm], writes=[rY])
                            if e_ == 0 and gi_ == 0:
                                ph.add("act", lambda e, dc=dc, pY=pY, tsl=tsl: e.activation(out=yacc[:, dc, tsl], in_=pY[:], func=AF.Copy),
                                       [rY], [self._ry(tq, dc)])
                            else:
                                ph.add("dve", lambda e, dc=dc, pY=pY, tsl=tsl: e.tensor_tensor(yacc[:, dc, tsl], yacc[:, dc, tsl], pY[:], ALU.add),
                                       [rY, self._ry(tq, dc)], [self._ry(tq, dc)])
            for sub in range(HALF // SW):
                tok = hf * HALF + sub * SW
                ht, rh = htp.next()
                ph.dma("sp", ht[:], hv[:, :, tok:tok + SW], lh, writes=[rh])
                rys = [self._ry((sub * SW) // 512, dc) for dc in range(8)]
                ryall = Res()
                ryall.writers = [w for r in rys for w in r.writers]
                self.post_norm_residual(ph, yacc[:, :, sub * SW:(sub + 1) * SW], ryall, ht[:], rh, layer, 2, ht[:], rh, wk, width=SW)
                for r in rys:
                    r.readers.extend(ryall.readers)
                if layer == 0:
                    ph.dma("sp", hd[:, :, tok:tok + SW], ht[:], lst, reads=[rh])
                else:
                    for s2 in range(SW // 128):
                        ot, rot = otp.next()
                        for hh in range(2):
                            pX, rX = pa.next()
                            for c4 in range(4):
                                c = hh * 4 + c4
                                ph.tr(pX[:, c4 * 128:(c4 + 1) * 128], ht[:, c, s2 * 128:(s2 + 1) * 128], self.ident_f[:], reads=[rh], writes=[rX])
                            if hh == 0:
                                ph.add("act", lambda e, ot=ot, pX=pX: e.activation(out=ot[:, 0:512], in_=pX[:], func=AF.Copy), [rX], [rot])
                            else:
                                ph.add("dve", lambda e, ot=ot, pX=pX: e.tensor_copy(ot[:, 512:1024], pX[:]), [rX], [rot], nowaw=True)
                        ph.dma("sp", self.out[tok + s2 * 128: tok + (s2 + 1) * 128, :], ot[:], lot[s2 % 2], reads=[rot])
            self._rycache = {}
        ph.emit()


def _ry(self, tq, dc):
    if not hasattr(self, "_rycache"):
        self._rycache = {}
    key = (tq, dc)
    if key not in self._rycache:
        self._rycache[key] = Res()
    return self._rycache[key]


Prog.ph_ffn = ph_ffn
Prog._ry = _ry


def ph_dil(self):
    nc, K, I, Dm = self.nc, self.K, self.I, self.Dm
    gd = Dm["gd"]
    with ExitStack() as es:
        ph = Phase(K, "dil")
        Qn = [self.sb(es, "Qn%d" % i, [128, S], BF16) for i in range(2)]
        Kn = [self.sb(es, "Kn%d" % i, [128, S], BF16) for i in range(2)]
        Qd = [[self.sb(es, "Qd%d_%d" % (i, g), [128, S], BF16) for g in range(2)] for i in range(2)]
        Kd = [[self.sb(es, "Kd%d_%d" % (i, g), [128, S], BF16) for g in range(2)] for i in range(2)]
        Vd = [[self.sb(es, "Vd%d_%d" % (i, g), [128, 32 * 65 + 64], BF16) for g in range(3)] for i in range(2)]
        Td = [self.sb(es, "Td%d" % i, [128, 3, 256], BF16) for i in range(2)]
        rQ, rK, rV, rT = [Res(), Res()], [Res(), Res()], [Res(), Res()], [Res(), Res()]
        rQd = [[Res(), Res()], [Res(), Res()]]
        rKd = [[Res(), Res()], [Res(), Res()]]
        lq, lk, lv, lt, lo = ([K.lane(), K.lane()] for _ in range(5))
        Oacc = [self.sb(es, "Oacc%d" % i, [128, S], F32) for i in range(2)]
        rOa = [Res(), Res()]
        ptp = Pool2([self.sb(es, "Pt%d" % i, [128, 256], BF16) for i in range(8)])
        psS = Pool2([self.ps(es, "psS%d" % i) for i in range(4)])
        obanks = [self.ps(es, "psO%d" % i) for i in range(3)]
        rOb = [Res() for _ in range(3)]
        psL = Pool2([self.ps(es, "psL0")])
        rinvp = Pool2([self.sb(es, "rinv%d" % i, [128, 512], F32) for i in range(2)])
        osbp = Pool2([self.sb(es, "osb%d" % i, [128, 512], BF16) for i in range(2)])
        for b in range(2):
            ph.v("pool", "memset", Qn[b][64:128, :], 0.0, writes=[rQ[b]])
            ph.v("pool", "memset", Kn[b][64:128, :], 0.0, writes=[rK[b]])
            for g in range(2):
                ph.v("pool", "memset", Qd[b][g][64:128, :], 0.0, writes=[rQd[b][g]])
                ph.v("pool", "memset", Kd[b][g][64:128, :], 0.0, writes=[rKd[b][g]])
            for g in range(3):
                ph.v("pool", "memset", Vd[b][g][:, 2080:2144], 0.0, writes=[rV[b]], nowaw=(g > 0))
        kk = 0
        LA = 5

        def prep_loads(h):
            b = h % 2
            ph.dma("sp", Qn[b][0:64, :], Dm["qkT"][h * 64:(h + 1) * 64, :], lq[b], writes=[rQ[b]])
            ph.dma("sp", Kn[b][0:64, :], Dm["qkT"][1024 + h * 64:1024 + (h + 1) * 64, :], lk[b], writes=[rK[b]])
            for g in range(3):
                ph.dma("sp", Vd[b][g][:, 0:2080], Dm["vaug"][g][h], lv[b], writes=[rV[b]], nowaw=(g > 0))
            ph.dma("sp", Td[b][:], AP(gd.tensor, h * 3 * 128 * (LD + 1) + 127, [[LD, 128], [128 * (LD + 1), 3], [1, 256]]), lt[b],
                   writes=[rT[b]])

        def prep_chunks(h):
            b = h % 2
            ch = []
            for g in (1, 2):
                dil = DILS[g]

                def cq(g=g, dil=dil):
                    ph.v("dve", "tensor_copy", Qd[b][g - 1][0:64, :].rearrange("p (r n) -> p r n", r=dil),
                         Qn[b][0:64, :].rearrange("p (n r) -> p r n", r=dil), reads=[rQ[b]], writes=[rQd[b][g - 1]])

                def ck(g=g, dil=dil):
                    ph.add("act", lambda e: e.activation(out=Kd[b][g - 1][0:64, :].rearrange("p (r n) -> p r n", r=dil),
                                                         in_=Kn[b][0:64, :].rearrange("p (n r) -> p r n", r=dil), func=AF.Copy),
                           [rK[b]], [rKd[b][g - 1]])
                ch.append(cq)
                ch.append(ck)
            return ch

        def sweep(h, chunks):
            nonlocal kk
            b = h % 2
            items = []
            nbank = 0
            for g in range(3):
                dil = DILS[g]
                nb = (S // dil) // 128
                for r in range(dil):
                    for kb in range(nb):
                        items.append((g, r, kb, nbank))
                nbank += (dil * nb) // 4
            st = {}
            fe = {0: True, 1: True, 2: True}

            def stage1(it, b=b):
                nonlocal kk
                g, r, kb, nbk = it
                dil = DILS[g]
                nb = (S // dil) // 128
                Qs, rQs = (Qn[b], rQ[b]) if g == 0 else (Qd[b][g - 1], rQd[b][g - 1])
                Ks, rKs = (Kn[b], rK[b]) if g == 0 else (Kd[b][g - 1], rKd[b][g - 1])
                qb = r * nb + kb
                m0 = qb * 128
                N = 256 if kb + 1 < nb else 128
                pS, rS = psS.next()
                ph.mm(pS[:, 0:N], Ks[:, m0:m0 + 128], Qs[:, m0:m0 + N], True, True, reads=[rKs, rQs], writes=[rS])
                Pt, rP = ptp.next()
                ph.act(Pt[:, 0:N], pS[:, 0:N], AF.Exp, reads=[rS], writes=[rP])
                ph.v("pool" if kk % 3 == 2 else "dve", "tensor_tensor", Pt[:, 0:N], Pt[:, 0:N], Td[b][:, g, 0:N], ALU.mult,
                     reads=[rP, rT[b]], writes=[rP])
                kk += 1
                st[it] = (Pt, rP, N)

            def stage2(it, b=b):
                g, r, kb, nbk = it
                dil = DILS[g]
                Ls = S // dil
                nb = Ls // 128
                qb = r * nb + kb
                Pt, rP, N = st.pop(it)
                bi = (nbk + qb // 4) % 3
                co = (qb % 4) * 128
                ph.mm(obanks[bi][:, co:co + 128], Vd[b][g][:, qb * 65:qb * 65 + 128], Pt[:, 0:128], kb == 0, True,
                      reads=[rV[b], rP], writes=[rOb[bi]])
                if N == 256:
                    bi2 = (nbk + (qb + 1) // 4) % 3
                    co2 = ((qb + 1) % 4) * 128
                    ph.mm(obanks[bi2][:, co2:co2 + 128], Vd[b][g][:, qb * 65:qb * 65 + 128], Pt[:, 128:256], True, False,
                          reads=[rV[b], rP], writes=[rOb[bi2]])
                if qb % 4 == 3:
                    M0 = (qb // 4) * 512
                    bank = obanks[bi]
                    if g == 0:
                        ph.add("act", lambda e, bank=bank, M0=M0, b=b: e.activation(out=Oacc[b][0:65, M0:M0 + 512], in_=bank[0:65, :],
                                                                                  func=AF.Copy), [rOb[bi]], [rOa[b]], nowaw=not fe[g])
                    else:
                        ov = Oacc[b][0:65, :].rearrange("p (n r) -> p r n", r=dil)
                        r0, n0 = M0 // Ls, M0 % Ls
                        if Ls >= 512:
                            dst = ov[:, r0, n0:n0 + 512]
                            src = bank[0:65, :]
                        else:
                            a = 512 // Ls
                            dst = ov[:, r0:r0 + a, 0:Ls]
                            src = bank[0:65, :].rearrange("p (a n) -> p a n", a=a)
                        ph.add("dve", lambda e, dst=dst, src=src: e.tensor_tensor(dst, dst, src, ALU.add), [rOb[bi], rOa[b]], [rOa[b]],
                               nowaw=not fe[g])
                    fe[g] = False

            for i in range(len(items) + LA):
                if i < len(items):
                    stage1(items[i])
                if i >= LA:
                    stage2(items[i - LA])
                if chunks and i % 8 == 5:
                    chunks.pop(0)()
            while chunks:
                chunks.pop(0)()
            for qt in range(NT):
                self.finish_chunk(ph, None, None, Oacc[b], rOa[b], qt, h, psL, rinvp, osbp, lo, first=False, src_is_sbuf=True)

        prep_loads(0)
        for c_ in prep_chunks(0):
            c_()
        for h in range(16):
            nxt = []
            if h + 1 < 16:
                prep_loads(h + 1)
                nxt = prep_chunks(h + 1)
            sweep(h, nxt)
        ph.emit()


Prog.ph_dil = ph_dil


NBK = 23
BK = 512
NSLOT = NBK * BK
I32 = mybir.dt.int32


def ph_moe(self):
    nc, K, I, Dm = self.nc, self.K, self.I, self.Dm
    layer = 1
    hv = Dm["hT3"].rearrange("(c p) t -> p c t", p=128)
    xg = self.dscr("xg", [NSLOT, D], BF16)
    yg = self.dscr("yg", [NSLOT, D], F32)
    htm_d = self.dscr("h3tm", [S, D], F32)
    es0 = self.es
    idxG = self.sb(es0, "idxG", [128, 32, 2], I32)
    pk = self.sb(es0, "pk", [128, 32, 2], F32)
    idxW = self.sb(es0, "idxW", [128, NBK, 32], I32)
    idxD = self.sb(es0, "idxD", [128, NBK, 28], I32)
    SW = 256
    with ExitStack() as es:
        ph = Phase(K, "moe_a")
        wk = self.make_wk(es, ph, width=SW)
        l0 = K.lane()
        Wr, rWr = self.load_weight_bf16(ph, es, "Wr", I["router"], D, 8, K.lane())
        tri = self.sb(es, "tri", [128, 128], BF16)
        onesb = self.sb(es, "onesb", [128, 128], BF16)
        c1 = self.sb(es, "c1", [128, 32], F32)
        c2 = self.sb(es, "c2", [128, 28], F32)
        jv = self.sb(es, "jv", [128, NBK], F32)
        rc = Res()
        ph.dma("sp", tri[:], I["tri"], l0, writes=[rc], nowaw=True)
        ph.dma("sp", c1[:], I["c1"], l0, writes=[rc], nowaw=True)
        ph.dma("sp", c2[:], I["c2"], l0, writes=[rc], nowaw=True)
        ph.dma("sp", jv[:], I["jv"][:, 0:NBK], l0, writes=[rc], nowaw=True)
        ph.v("pool", "memset", onesb[:], 1.0, writes=[rc], nowaw=True)
        carry = self.sb(es, "carry", [128, 8], F32)
        rcar = Res()
        ph.v("dve", "memset", carry[:], 0.0, writes=[rcar])
        htp = Pool2([self.sb(es, "hts%d" % i, [128, 8, SW], F32) for i in range(2)])
        lh = [K.lane(), K.lane()]
        utp = Pool2([self.sb(es, "uts%d" % i, [128, 8, SW], BF16) for i in range(2)])
        utm_all = self.sb(es, "utm_all", [128, 32, D], BF16)
        rum = Res()
        lsc = [K.lane(), K.lane()]
        htm_p = Pool2([self.sb(es, "htm%d" % i, [128, D], F32) for i in range(2)])
        lht = [K.lane(), K.lane()]
        psU = Pool2([self.ps(es, "psU%d" % i, [128, D], BF16) for i in range(2)])
        psH = Pool2([self.ps(es, "psH%d" % i) for i in range(2)])
        psR = Pool2([self.ps(es, "psR")])
        psP = Pool2([self.ps(es, "psP")])
        lg = self.sb(es, "lg", [128, 8], F32)
        mx = self.sb(es, "mx", [128, 8], F32)
        dd = self.sb(es, "dd", [128, 1], F32)
        mm2 = self.sb(es, "mm2", [128, 32, 2, 8], F32)
        Mb = self.sb(es, "Mb", [128, 8], BF16)
        posf = self.sb(es, "posf", [128, 32, 8], F32)
        rl = Res()
        rpos = Res()
        def load_h(sub):
            ht, rh = htp.next()
            ph.dma("sp", ht[:], hv[:, :, sub * SW:(sub + 1) * SW], lh[sub % 2], writes=[rh])
            return ht, rh

        nxt_h = load_h(0)
        for sub in range(S // SW):
            tok = sub * SW
            ht, rh = nxt_h
            if sub + 1 < S // SW:
                nxt_h = load_h(sub + 1)
            ut, ru = utp.next()
            self.norm_modulate(ph, ht[:], rh, layer, 2, ut[:], ru, wk, width=SW)
            for s2 in range(SW // 128):
                i = (tok // 128) + s2
                cs = slice(s2 * 128, (s2 + 1) * 128)
                htm, rhm = htm_p.next()
                for hh in range(2):
                    pX, rX = psH.next()
                    for c4 in range(4):
                        ph.tr(pX[:, c4 * 128:(c4 + 1) * 128], ht[:, hh * 4 + c4, cs], self.ident_f[:], reads=[rh], writes=[rX])
                    if hh == 0:
                        ph.add("act", lambda e, htm=htm, pX=pX: e.activation(out=htm[:, 0:512], in_=pX[:], func=AF.Copy), [rX], [rhm])
                    else:
                        ph.add("dve", lambda e, htm=htm, pX=pX: e.tensor_copy(htm[:, 512:1024], pX[:]), [rX], [rhm], nowaw=True)
                ph.dma("sp", htm_d[i * 128:(i + 1) * 128, :], htm[:], lht[i % 2], reads=[rhm])
                pU, rU = psU.next()
                for c in range(8):
                    ph.tr(pU[:, c * 128:(c + 1) * 128], ut[:, c, cs], self.ident_b[:], reads=[ru], writes=[rU])
                ph.add("act", lambda e, i=i, pU=pU: e.activation(out=utm_all[:, i, :], in_=pU[:], func=AF.Copy), [rU], [rum], nowaw=True)
                pR, rR = psR.next()
                for c in range(8):
                    ph.mm(pR[:, 0:8], ut[:, c, cs], Wr[:, c, 0:8], c == 0, c == 7, reads=[ru, rWr], writes=[rR])
                ph.v("dve", "tensor_copy", lg[:], pR[:, 0:8], reads=[rR], writes=[rl])
                ph.v("dve", "max", mx[:], lg[:], reads=[rl], writes=[rl])
                ph.v("dve", "tensor_tensor", dd[:], mx[:, 0:1], mx[:, 1:2], ALU.subtract, reads=[rl], writes=[rl])
                ph.add("act", lambda e, i=i: e.activation(out=pk[:, i, 0:1], in_=dd[:], func=AF.Sigmoid), [rl], [rl])
                ph.add("act", lambda e, i=i: e.activation(out=pk[:, i, 1:2], in_=dd[:], func=AF.Sigmoid, scale=-1.0), [rl], [rl])
                ph.add("dve", lambda e, i=i: e.tensor_scalar(mm2[:, i, 0, :], lg[:], mx[:, 0:1], None, ALU.is_ge), [rl], [rl])
                ph.add("dve", lambda e, i=i: e.tensor_scalar(mm2[:, i, 1, :], lg[:], mx[:, 1:2], None, ALU.is_equal), [rl], [rl])
                ph.add("dve", lambda e, i=i: e.tensor_tensor(Mb[:], mm2[:, i, 0, :], mm2[:, i, 1, :], ALU.add), [rl], [rl])
                pP, rP = psP.next()
                ph.mm(pP[:, 0:8], tri[:], Mb[:], True, True, reads=[rl, rc], writes=[rP])
                ph.mm(pP[:, 8:16], onesb[:], Mb[:], True, True, reads=[rl, rc], writes=[rP])
                ph.add("dve", lambda e, i=i, pP=pP: e.tensor_tensor(posf[:, i, :], pP[:, 0:8], carry[:], ALU.add), [rP, rcar], [rpos], nowaw=True)
                ph.add("dve", lambda e, pP=pP: e.tensor_tensor(carry[:], carry[:], pP[:, 8:16], ALU.add), [rP, rpos], [rcar])
        nbk = self.sb(es, "nbk", [128, 8], F32)
        cum = self.sb(es, "cum", [128, 8], F32)
        off = self.sb(es, "off", [128, 8], F32)
        rb = Res()
        ph.v("dve", "tensor_scalar", nbk[:], carry[:], 0.0, None, ALU.is_gt, reads=[rcar], writes=[rb])
        for m in range(1, 8):
            ph.v("dve", "scalar_tensor_tensor", nbk[:], carry[:], float(BK * m), nbk[:], ALU.is_gt, ALU.add, reads=[rcar, rb], writes=[rb])
        ph.v("dve", "tensor_copy", cum[:, 0:1], nbk[:, 0:1], reads=[rb], writes=[rb])
        for e_ in range(1, 8):
            ph.v("dve", "tensor_tensor", cum[:, e_:e_ + 1], cum[:, e_ - 1:e_], nbk[:, e_:e_ + 1], ALU.add, reads=[rb], writes=[rb])
        ph.v("dve", "tensor_tensor", off[:], cum[:], nbk[:], ALU.subtract, reads=[rb], writes=[rb])
        ph.v("dve", "tensor_scalar", off[:], off[:], float(BK), None, ALU.mult, reads=[rb], writes=[rb])
        cmp_ = self.sb(es, "cmp", [128, NBK, 8], F32)
        eb = self.sb(es, "eb", [128, NBK], F32)
        ph.v("dve", "tensor_tensor", cmp_[:], cum[:, None, :].to_broadcast([128, NBK, 8]), jv[:, 0:NBK, None].to_broadcast([128, NBK, 8]),
             ALU.is_le, reads=[rb, rc], writes=[rb])
        ph.v("dve", "tensor_reduce", eb[:], cmp_[:], AX.X, ALU.add, reads=[rb], writes=[rb])
        ph.v("dve", "tensor_scalar", eb[:], eb[:], 7.0, None, ALU.min, reads=[rb], writes=[rb])
        ebw = self.sb(es, "ebw", [128, NBK, 2], F32)
        ph.add("dve", lambda e: e.tensor_scalar(ebw[:, :, 0], eb[:], 4096.0, None, ALU.mult), [rb], [rb])
        ph.add("dve", lambda e: e.tensor_scalar(ebw[:, :, 1], eb[:], 3584.0, None, ALU.mult), [rb], [rb])
        ridx = Res()
        ph.v("dve", "tensor_tensor", idxW[:], c1[:, None, :].to_broadcast([128, NBK, 32]), ebw[:, :, 0:1].to_broadcast([128, NBK, 32]),
             ALU.add, reads=[rb, rc], writes=[ridx])
        ph.v("dve", "tensor_tensor", idxD[:], c2[:, None, :].to_broadcast([128, NBK, 28]), ebw[:, :, 1:2].to_broadcast([128, NBK, 28]),
             ALU.add, reads=[rb, rc], writes=[ridx], nowaw=True)
        sfa = self.sb(es, "sfa", [128, 32, 8], F32)
        t1a = self.sb(es, "t1a", [128, 32, 2, 8], F32)
        ssel = self.sb(es, "ssel", [128, 32, 2], F32)
        ph.v("dve", "tensor_tensor", sfa[:], posf[:], off[:, None, :].to_broadcast([128, 32, 8]), ALU.add, reads=[rpos, rb], writes=[rb])
        ph.v("dve", "tensor_tensor", t1a[:], mm2[:], sfa[:, :, None, :].to_broadcast([128, 32, 2, 8]), ALU.mult, reads=[rb, rl], writes=[rb])
        ph.v("dve", "tensor_reduce", ssel[:], t1a[:], AX.X, ALU.add, reads=[rb], writes=[rb])
        ph.v("dve", "tensor_copy", idxG[:], ssel[:], reads=[rb], writes=[ridx], nowaw=True)
        for i in range(32):
            for k in range(2):
                ph.add("pool", lambda e, i=i, k=k: e.indirect_dma_start(
                    out=xg[:, :], out_offset=bass.IndirectOffsetOnAxis(ap=idxG[:, i, k:k + 1], axis=0),
                    in_=utm_all[:, i, :], in_offset=None), [rum, ridx], [], lane=lsc[i % 2])
        if self.debug:
            dbc = self.dscr("dbg_counts", [128, 8], F32, dbg=True)
            ph.dma("sp", dbc, carry[:], l0, reads=[rcar])
        ph.emit()
    Wgv = I["exp_g"].rearrange("e k (g j) -> (e k g) j", j=896)
    Wuv = I["exp_u"].rearrange("e k (g j) -> (e k g) j", j=896)
    Wdv = I["exp_d"].rearrange("e f d -> (e f) d")
    with ExitStack() as es:
        ph = Phase(K, "moe_b")
        xT = [self.sb(es, "xT%d" % i, [128, 8, BK], BF16) for i in range(2)]
        rxT = [Res(), Res()]
        yaccp = [self.sb(es, "yacc%d" % i, [128, 8, BK], F32) for i in range(2)]
        wgp = [self.sb(es, "wg%d" % i, [128, 8, 896], BF16) for i in range(2)]
        wup = [self.sb(es, "wu%d" % i, [128, 8, 896], BF16) for i in range(2)]
        wdp = [self.sb(es, "wd%d" % i, [128, 7, D], BF16) for i in range(2)]
        rw = [Res(), Res()]
        lw = [K.lane(), K.lane()]
        hmp = Pool2([self.sb(es, "hmid%d" % i, [128, 7, BK], BF16) for i in range(2)])
        sp_ = Pool2([self.sb(es, "ssb%d" % i, [128, BK], BF16) for i in range(2)])
        xtl = Pool2([self.sb(es, "xtl%d" % i, [128, D], BF16) for i in range(3)])
        lx = [K.lane() for _ in range(3)]
        ytl = Pool2([self.sb(es, "ytl%d" % i, [128, D], F32) for i in range(2)])
        ly = [K.lane(), K.lane()]
        pa = Pool2([self.ps(es, "pa%d" % i) for i in range(2)])
        pbk = Pool2([self.ps(es, "pb%d" % i) for i in range(2)])
        pyk = Pool2([self.ps(es, "py%d" % i) for i in range(2)])
        pxu = Pool2([self.ps(es, "pxu", [128, D], BF16)])
        pyt = Pool2([self.ps(es, "pyt")])
        wcount = 0
        nx = 0

        def load_x(b_):
            nonlocal nx
            xb = xT[b_ % 2]
            first = True
            for j in range(BK // 128):
                xt_, rx_ = xtl.next()
                ph.dma("sp", xt_[:], xg[b_ * BK + j * 128: b_ * BK + (j + 1) * 128, :], lx[nx % 3], writes=[rx_])
                nx += 1
                pU, rU = pxu.next()
                for c in range(8):
                    ph.tr(pU[:, c * 128:(c + 1) * 128], xt_[:, c * 128:(c + 1) * 128], self.ident_b[:], reads=[rx_], writes=[rU])
                src = pU[:].rearrange("p (c s) -> p c s", c=8)
                dst = xb[:, :, j * 128:(j + 1) * 128]
                ph.add("act", lambda e, dst=dst, src=src: e.activation(out=dst, in_=src, func=AF.Copy), [rU], [rxT[b_ % 2]], nowaw=not first)
                first = False

        def load_w(b_, g4):
            nonlocal wcount
            wb = wcount % 2
            wcount += 1
            first = True
            for c in range(8):
                for Wv, wt in ((Wgv, wgp[wb]), (Wuv, wup[wb])):
                    ph.add("pool", lambda e, wt=wt, Wv=Wv, b_=b_, c=c, g4=g4: e.indirect_dma_start(
                        out=wt[:, c, :], out_offset=None, in_=Wv[:, :],
                        in_offset=bass.IndirectOffsetOnAxis(ap=idxW[:, b_, c * 4 + g4:c * 4 + g4 + 1], axis=0)),
                        [], [rw[wb]], lane=lw[wb], nowaw=not first)
                    first = False
            for j in range(7):
                ph.add("pool", lambda e, wb=wb, b_=b_, j=j, g4=g4: e.indirect_dma_start(
                    out=wdp[wb][:, j, :], out_offset=None, in_=Wdv[:, :],
                    in_offset=bass.IndirectOffsetOnAxis(ap=idxD[:, b_, g4 * 7 + j:g4 * 7 + j + 1], axis=0)),
                    [], [rw[wb]], lane=lw[wb], nowaw=True)
            return wb

        rydp = [[Res() for _ in range(8)] for _ in range(2)]
        load_x(0)
        pending = load_w(0, 0)
        for b_ in range(NBK):
            xb, rxb = xT[b_ % 2], rxT[b_ % 2]
            yacc = yaccp[b_ % 2]
            ryd = rydp[b_ % 2]
            for g4 in range(4):
                wb = pending
                if g4 < 3:
                    pending = load_w(b_, g4 + 1)
                elif b_ + 1 < NBK:
                    pending = load_w(b_ + 1, 0)
                if g4 == 1 and b_ + 1 < NBK:
                    load_x(b_ + 1)
                wg, wu, wd = wgp[wb], wup[wb], wdp[wb]
                hmid, rhm_ = hmp.next()
                for j in range(7):
                    pA, rA = pa.next()
                    pB, rB = pbk.next()
                    for c in range(8):
                        ph.mm(pA[:], wg[:, c, j * 128:(j + 1) * 128], xb[:, c, :], c == 0, c == 7, reads=[rw[wb], rxb], writes=[rA])
                    for c in range(8):
                        ph.mm(pB[:], wu[:, c, j * 128:(j + 1) * 128], xb[:, c, :], c == 0, c == 7, reads=[rw[wb], rxb], writes=[rB])
                    ss, rs = sp_.next()
                    ph.act(ss[:], pA[:], AF.Silu, reads=[rA], writes=[rs])
                    ph.add("dve", lambda e, j=j, ss=ss, pB=pB, hmid=hmid: e.tensor_tensor(hmid[:, j, :], ss[:], pB[:], ALU.mult),
                           [rs, rB], [rhm_], nowaw=(j > 0))
                for dc in range(8):
                    pY, rY = pyk.next()
                    for j in range(7):
                        ph.mm(pY[:], wd[:, j, dc * 128:(dc + 1) * 128], hmid[:, j, :], j == 0, j == 6, reads=[rw[wb], rhm_], writes=[rY])
                    if g4 == 0:
                        ph.add("act", lambda e, dc=dc, pY=pY, yacc=yacc: e.activation(out=yacc[:, dc, :], in_=pY[:], func=AF.Copy), [rY], [ryd[dc]])
                    else:
                        ph.add("dve", lambda e, dc=dc, pY=pY, yacc=yacc: e.tensor_tensor(yacc[:, dc, :], yacc[:, dc, :], pY[:], ALU.add),
                               [rY, ryd[dc]], [ryd[dc]])
            for j in range(BK // 128):
                yt_, ryt = ytl.next()
                for hh in range(2):
                    pX, rX = pyt.next()
                    for c4 in range(4):
                        c = hh * 4 + c4
                        ph.tr(pX[:, c4 * 128:(c4 + 1) * 128], yacc[:, c, j * 128:(j + 1) * 128], self.ident_f[:], reads=[ryd[c]], writes=[rX])
                    ph.add("act", lambda e, yt_=yt_, pX=pX, hh=hh: e.activation(out=yt_[:, hh * 512:(hh + 1) * 512], in_=pX[:], func=AF.Copy),
                           [rX], [ryt], nowaw=(hh > 0))
                ph.dma("sp", yg[b_ * BK + j * 128: b_ * BK + (j + 1) * 128, :], yt_[:], ly[j % 2], reads=[ryt])
        ph.emit()
    with ExitStack() as es:
        ph = Phase(K, "moe_c")
        selE = self.sb(es, "selE", [8, 8, 128], F32)
        rse = Res()
        ph.v("dve", "tensor_copy", selE[:], self.ident_f[0:8, 0:8, None].to_broadcast([8, 8, 128]), writes=[rse])
        pg = [self.ps(es, "pg%d" % i) for i in range(2)]
        rpg = Res()
        ggT = self.sb(es, "ggT", [8, 128], F32)
        rgt = Res()
        ph.tr(pg[0][0:8, 0:128], self.vecs[:, 1, 5, :], self.ident_f[:], writes=[rpg])
        ph.act(ggT[:], pg[0][0:8, 0:128], AF.Copy, reads=[rpg], writes=[rgt])
        ggbc = self.sb(es, "ggbc", [128, D], F32)
        rgb = Res()
        for c in range(8):
            ph.mm(pg[c // 4][:, (c % 4) * 128:(c % 4 + 1) * 128], selE[0:8, c, :], ggT[0:8, :], True, True, reads=[rse, rgt, rpg], writes=[rpg])
        for hh in range(2):
            ph.add("act", lambda e, hh=hh: e.activation(out=ggbc[:, hh * 512:(hh + 1) * 512], in_=pg[hh][:], func=AF.Copy), [rpg], [rgb], nowaw=(hh > 0))
        eps_t = self.sb(es, "eps_t", [128, 1], F32)
        ph.v("pool", "memset", eps_t[:], EPS, writes=[rgb], nowaw=True)
        yap = Pool2([self.sb(es, "ya%d" % i, [128, D], F32) for i in range(3)])
        ybp = Pool2([self.sb(es, "yb%d" % i, [128, D], F32) for i in range(3)])
        hmp2 = Pool2([self.sb(es, "hm%d" % i, [128, D], F32) for i in range(3)])
        jk = Pool2([self.sb(es, "jk%d" % i, [128, D], BF16) for i in range(2)])
        ssp = Pool2([self.sb(es, "ss%d" % i, [128, 2], F32) for i in range(2)])
        la, lb_, lh2, lo2 = ([K.lane(), K.lane(), K.lane()] for _ in range(4))
        def fetch(i):
            ya, rya = yap.next()
            yb, ryb = ybp.next()
            hm, rhm2 = hmp2.next()
            ph.add("pool", lambda e, i=i, ya=ya: e.indirect_dma_start(
                out=ya[:, :], out_offset=None, in_=yg[:, :], in_offset=bass.IndirectOffsetOnAxis(ap=idxG[:, i, 0:1], axis=0)),
                [], [rya], lane=la[i % 3])
            ph.add("pool", lambda e, i=i, yb=yb: e.indirect_dma_start(
                out=yb[:, :], out_offset=None, in_=yg[:, :], in_offset=bass.IndirectOffsetOnAxis(ap=idxG[:, i, 1:2], axis=0)),
                [], [ryb], lane=lb_[i % 3])
            ph.dma("sp", hm[:], htm_d[i * 128:(i + 1) * 128, :], lh2[i % 3], writes=[rhm2])
            return ya, rya, yb, ryb, hm, rhm2

        fq = [fetch(0), fetch(1)]
        for i in range(32):
            ya, rya, yb, ryb, hm, rhm2 = fq.pop(0)
            if i + 2 < 32:
                fq.append(fetch(i + 2))
            ph.add("dve", lambda e, i=i, ya=ya: e.tensor_scalar(ya[:], ya[:], pk[:, i, 0:1], None, ALU.mult), [rya], [rya])
            ph.add("dve", lambda e, i=i, ya=ya, yb=yb: e.scalar_tensor_tensor(ya[:], yb[:], pk[:, i, 1:2], ya[:], ALU.mult, ALU.add),
                   [rya, ryb], [rya])
            j_, rj = jk.next()
            ss, rss = ssp.next()
            ph.add("act", lambda e, ya=ya, j_=j_, ss=ss: e.activation(out=j_[:], in_=ya[:], func=AF.Square, accum_out=ss[:, 0:1]),
                   [rya], [rj, rss])
            ph.add("act", lambda e, ss=ss: e.activation(out=ss[:, 1:2], in_=ss[:, 0:1], func=AF.Sqrt, bias=eps_t[:, 0:1], scale=1.0 / D),
                   [rss, rgb], [rss])
            ph.add("dve", lambda e, ss=ss: e.reciprocal(ss[:, 1:2], ss[:, 1:2]), [rss], [rss])
            ph.add("dve", lambda e, ya=ya, ss=ss: e.scalar_tensor_tensor(ya[:], ya[:], ss[:, 1:2], ggbc[:], ALU.mult, ALU.mult),
                   [rya, rss, rgb], [rya])
            ph.add("pool", lambda e, ya=ya, hm=hm: e.tensor_tensor(hm[:], hm[:], ya[:], ALU.add), [rya, rhm2], [rhm2])
            ph.dma("sp", self.out[i * 128:(i + 1) * 128, :], hm[:], lo2[i % 3], reads=[rhm2])
        ph.emit()


Prog.ph_moe = ph_moe
```

```python
import numpy as np
import ml_dtypes
from contextlib import ExitStack
import concourse.bass as bass
import concourse.mybir as mybir
from concourse.ap import AP
from concourse.bass_utils import run_bass_kernel_spmd

F32 = mybir.dt.float32
BF16 = mybir.dt.bfloat16
ALU = mybir.AluOpType
AF = mybir.ActivationFunctionType
AX = mybir.AxisListType

S = 4096
D = 1024
NT = 8
EPS = 1e-6
LC = 3327
WC = 3200
LD = 383
WD = 256
NEG = -30000.0
DILS = (1, 4, 16)


class Res:
    __slots__ = ("writers", "readers", "prev_readers")

    def __init__(self):
        self.writers = []
        self.readers = []
        self.prev_readers = []


class Lane:
    def __init__(self, sem):
        self.sem = sem
        self.count = 0


class Ins:
    __slots__ = ("eng", "fn", "deps", "sig", "cnt", "lane", "lane_val", "idx")


class Ctx:
    def __init__(self, nc, es):
        self.nc = nc
        self.es = es
        self.engs = {"pe": nc.tensor, "act": nc.scalar, "dve": nc.vector, "pool": nc.gpsimd, "sp": nc.sync}
        self.esem = {k: es.enter_context(nc.semaphore("es_" + k)) for k in ("pe", "act", "dve", "pool")}
        self.ecount = {k: 0 for k in self.esem}
        self.lanes = []
        self.nlane = 0

    def lane(self):
        if getattr(self, "free_lanes", None):
            ln = self.free_lanes.pop()
        else:
            ln = Lane(self.es.enter_context(self.nc.semaphore("ln%d" % self.nlane)))
            self.nlane += 1
            self.lanes.append(ln)
        if not hasattr(self, "phase_lanes"):
            self.phase_lanes = []
        self.phase_lanes.append(ln)
        return ln

    def recycle(self):
        if not hasattr(self, "free_lanes"):
            self.free_lanes = []
        self.free_lanes.extend(getattr(self, "phase_lanes", []))
        self.phase_lanes = []


class Phase:
    def __init__(self, K, name):
        self.K = K
        self.name = name
        self.streams = {k: [] for k in ("pe", "act", "dve", "pool", "sp")}
        self.used_lanes = {}

    def add(self, eng, fn, reads=(), writes=(), lane=None, nowaw=False):
        ins = Ins()
        ins.eng = eng
        ins.fn = fn
        ins.sig = False
        ins.cnt = None
        ins.lane = lane
        ins.lane_val = None
        if lane is not None:
            lane.count += 1
            ins.lane_val = 16 * lane.count
            self.used_lanes[id(lane)] = (lane, eng)
        deps = []
        for r in reads:
            deps.extend(r.writers)
        for w in writes:
            deps.extend(w.readers)
            if not nowaw:
                deps.extend(w.writers)
            else:
                deps.extend(w.prev_readers)
        dd = []
        seen = set()
        for dpi in deps:
            if id(dpi) in seen or dpi is ins:
                continue
            seen.add(id(dpi))
            if dpi.lane is None and dpi.eng == "pe" and eng == "pe":
                continue
            dd.append(dpi)
            if dpi.lane is None:
                dpi.sig = True
        ins.deps = dd
        for r in reads:
            r.readers.append(ins)
        for w in writes:
            if nowaw:
                w.writers.append(ins)
                w.prev_readers = w.prev_readers + w.readers
            else:
                w.writers = [ins]
                w.prev_readers = w.readers
            w.readers = []
        ins.idx = len(self.streams[eng])
        self.streams[eng].append(ins)
        return ins

    def emit(self):
        K = self.K
        nc = K.nc
        for e in ("pe", "act", "dve", "pool"):
            for ins in self.streams[e]:
                if ins.lane is None and ins.sig:
                    K.ecount[e] += 1
                    ins.cnt = K.ecount[e]
        streams = self.streams
        used = list(self.used_lanes.values())

        def run(ename, eng):
            waited = {}
            for ins in streams[ename]:
                need = {}
                for dpi in ins.deps:
                    if dpi.lane is not None:
                        key, val, sem = ("L", id(dpi.lane)), dpi.lane_val, dpi.lane.sem
                    else:
                        key, val, sem = ("E", dpi.eng), dpi.cnt, K.esem[dpi.eng]
                    if val > need.get(key, (0, None))[0]:
                        need[key] = (val, sem)
                for key, (val, sem) in need.items():
                    if waited.get(key, 0) >= val:
                        continue
                    eng.wait_ge(sem, val)
                    waited[key] = val
                bi = ins.fn(eng)
                if ins.lane is not None:
                    bi.then_inc(ins.lane.sem, 16)
                elif ins.sig:
                    bi.then_inc(K.esem[ename], 1)
            for lane, le in used:
                if le == ename:
                    eng.wait_ge(lane.sem, 16 * lane.count)

        with nc.Block() as block:
            @block.tensor
            def _(e):
                run("pe", e)

            @block.scalar
            def _(e):
                run("act", e)

            @block.vector
            def _(e):
                run("dve", e)

            @block.gpsimd
            def _(e):
                run("pool", e)

            @block.sync
            def _(e):
                run("sp", e)
        K.recycle()


class Pool2:
    def __init__(self, tiles):
        self.tiles = tiles
        self.res = [Res() for _ in tiles]
        self.i = 0

    def next(self):
        j = self.i % len(self.tiles)
        self.i += 1
        return self.tiles[j], self.res[j]


def dram_view(t, offset, pattern):
    return AP(t.tensor, offset, pattern)


def _t5_bucket_np(dist):
    n = np.maximum(dist, 0)
    nf = np.maximum(n, 1).astype(np.float32)
    lr = np.log(nf / np.float32(16)) / np.float32(np.log(2048 / 16))
    large = 16 + (lr.astype(np.float32) * np.float32(16)).astype(np.int32)
    large = np.minimum(large, 31)
    return np.where(n < 16, n, large)


def make_consts():
    c = {}
    c["ident_f"] = np.eye(128, dtype=np.float32)
    c["ident_b"] = np.eye(128, dtype=np.float32).astype(ml_dtypes.bfloat16)
    oh = np.zeros((33, LC), np.float32)
    d = np.arange(LC) - 511
    bk = _t5_bucket_np(d)
    for m in range(LC):
        if d[m] >= 0:
            oh[bk[m], m] = 1.0
        else:
            oh[32, m] = 1.0
    c["oh_c"] = oh
    ohd = np.zeros((3, 33, LD + 1), np.float32)
    for g, dil in enumerate(DILS):
        rel = np.arange(LD + 1) - 127
        bk = _t5_bucket_np(rel * dil)
        for m in range(LD + 1):
            if 0 <= rel[m] <= 128:
                ohd[g, bk[m], m] = 1.0
            else:
                ohd[g, 32, m] = 1.0
    c["oh_d"] = ohd
    ind = np.zeros((16, S), np.float32)
    for j in range(16):
        ind[j, j * 256:(j + 1) * 256] = 1.0
    c["ind"] = ind.astype(ml_dtypes.bfloat16)
    c["tri"] = np.triu(np.ones((128, 128), np.float32), k=1).astype(ml_dtypes.bfloat16)
    p = np.arange(128, dtype=np.float32)[:, None]
    k = np.arange(32, dtype=np.float32)[None, :]
    c["c1"] = ((k // 4) * 128 + p) * 4 + (k % 4)
    c["c2"] = np.arange(28, dtype=np.float32)[None, :] * 128 + p
    c["jv"] = np.tile(np.arange(24, dtype=np.float32)[None, :], (128, 1))
    return c


class Prog:
    def __init__(self, stop_after=None, debug=False):
        self.stop_after = stop_after
        self.debug = debug
        self.nc = bass.Bass("TRN2", target_bir_lowering=False)
        self.es = ExitStack()
        self.K = Ctx(self.nc, self.es)
        self.dbg_outputs = []

    def din(self, name, shape, dt=F32):
        return self.nc.dram_tensor(name, list(shape), dt, kind="ExternalInput").ap()

    def dscr(self, name, shape, dt, dbg=False):
        if dbg and self.debug:
            self.dbg_outputs.append(name)
            return self.nc.dram_tensor(name, list(shape), dt, kind="ExternalOutput").ap()
        return self.nc.dram_tensor(name, list(shape), dt, kind="Internal").ap()

    def sb(self, es, name, shape, dt):
        self.uid = getattr(self, "uid", 0) + 1
        return es.enter_context(self.nc.sbuf_tensor("s%d_%s" % (self.uid, name), list(shape), dt))

    def ps(self, es, name, shape=(128, 512), dt=F32):
        self.uid = getattr(self, "uid", 0) + 1
        return es.enter_context(self.nc.psum_tensor("p%d_%s" % (self.uid, name), list(shape), dt))

    def build(self):
        nc = self.nc
        I = {}
        I["x"] = self.din("x", [S, D])
        I["c"] = self.din("c", [8, 128])
        I["mod_w"] = self.din("mod_w", [2, D, 6 * D])
        I["mod_b"] = self.din("mod_b", [96, 128])
        I["norm_g"] = self.din("norm_g", [64, 128])
        I["in_w0"] = self.din("in_w0", [D, 3080])
        I["fgb"] = self.din("fgb", [8, 1])
        I["out_w0"] = self.din("out_w0", [D, D])
        I["in_w1"] = self.din("in_w1", [D, 3072])
        I["out_w1"] = self.din("out_w1", [D, D])
        I["rbt"] = self.din("rbt", [32, 16])
        I["ffn_g"] = self.din("ffn_g", [1, D, 2816])
        I["ffn_u"] = self.din("ffn_u", [1, D, 2816])
        I["ffn_d"] = self.din("ffn_d", [1, 2816, D])
        I["router"] = self.din("router", [D, 8])
        I["exp_g"] = self.din("exp_g", [8, D, 3584])
        I["exp_u"] = self.din("exp_u", [8, D, 3584])
        I["exp_d"] = self.din("exp_d", [8, 3584, D])
        I["ident_f"] = self.din("ident_f", [128, 128])
        I["ident_b"] = self.din("ident_b", [128, 128], BF16)
        I["oh_c"] = self.din("oh_c", [33, LC])
        I["oh_d"] = self.din("oh_d", [3, 33, LD + 1])
        I["ind"] = self.din("ind", [16, S], BF16)
        I["tri"] = self.din("tri", [128, 128], BF16)
        I["c1"] = self.din("c1", [128, 32])
        I["c2"] = self.din("c2", [128, 28])
        I["jv"] = self.din("jv", [128, 24])
        self.I = I
        self.out = nc.dram_tensor("out", [S, D], F32, kind="ExternalOutput").ap()

        Dm = {}
        Dm["hT0"] = self.dscr("hT0", [D, S], F32, dbg=True)
        Dm["hT1"] = self.dscr("hT1", [D, S], F32, dbg=True)
        Dm["hT2"] = self.dscr("hT2", [D, S], F32, dbg=True)
        Dm["hT3"] = self.dscr("hT3", [D, S], F32, dbg=True)
        Dm["qkT"] = self.dscr("qkT", [2048, S], BF16, dbg=True)
        Dm["vaug"] = self.dscr("vaug", [3, 16, 128, 32 * 65], BF16, dbg=True)
        Dm["cparts"] = self.dscr("cparts", [8, 6, S], BF16, dbg=True)
        Dm["oT"] = self.dscr("oT", [D, S], BF16, dbg=True)
        self.spT = self.dscr("spT", [8, S], F32, dbg=True)
        Dm["yT"] = self.dscr("yT", [D, S], F32, dbg=True)
        Dm["gc"] = self.dscr("gc", [9, 128, LC], BF16)
        Dm["gd"] = self.dscr("gd", [16, 3, 128, LD + 1], BF16)
        self.Dm = Dm

        es = self.es
        self.vecs = self.sb(es, "vecs", [128, 2, 6, 8], F32)
        self.ident_f = self.sb(es, "identf", [128, 128], F32)
        self.ident_b = self.sb(es, "identb", [128, 128], BF16)
        self.ones_div = self.sb(es, "onesdiv", [128, 128], BF16)
        self.ones_f = self.sb(es, "onesf", [128, 128], F32)

        phases = [
            ("prep", lambda: self.ph_prep()),
            ("inproj0", lambda: self.ph_inproj(0)),
            ("cum", lambda: self.ph_cum()),
            ("fox", lambda: self.ph_attn01(False)),
            ("moba", lambda: self.ph_attn01(True)),
            ("outproj0", lambda: self.ph_outproj(0)),
            ("ffn0", lambda: self.ph_ffn(0)),
            ("inproj1", lambda: self.ph_inproj(1)),
            ("dil", lambda: self.ph_dil()),
            ("outproj1", lambda: self.ph_outproj(1)),
            ("ffn1", lambda: self.ph_moe()),
        ]
        for name, fn in phases:
            fn()
            if self.stop_after == name:
                break
        self.es.close()
        return nc


def _ph_mm(self, out, lhsT, rhs, start, stop, reads=(), writes=()):
    return self.add("pe", lambda e: e.matmul(out, lhsT, rhs, start=start, stop=stop), reads, writes)


def _ph_tr(self, out, in_, ident, reads=(), writes=()):
    return self.add("pe", lambda e: e.transpose(out, in_, ident), reads, writes)


def _ph_act(self, out, in_, func, reads=(), writes=(), bias=0.0, scale=1.0):
    return self.add("act", lambda e: e.activation(out=out, in_=in_, func=func, bias=bias, scale=scale), reads, writes)


def _ph_dma(self, eng, out, in_, lane, reads=(), writes=(), nowaw=False, **kw):
    return self.add(eng, lambda e: e.dma_start(out=out, in_=in_, **kw), reads, writes, lane=lane, nowaw=nowaw)


def _ph_v(self, eng, meth, *args, reads=(), writes=(), nowaw=False, **kw):
    return self.add(eng, lambda e: getattr(e, meth)(*args, **kw), reads, writes, nowaw=nowaw)


Phase.mm = _ph_mm
Phase.tr = _ph_tr
Phase.act = _ph_act
Phase.dma = _ph_dma
Phase.v = _ph_v


def ph_prep(self):
    nc, K, I, Dm = self.nc, self.K, self.I, self.Dm
    with ExitStack() as es:
        ph = Phase(K, "prep_a")
        l0 = K.lane()
        lw = [K.lane(), K.lane()]
        rc = Res()
        ph.dma("sp", self.ident_f[:], I["ident_f"], l0, writes=[rc], nowaw=True)
        ph.dma("sp", self.ident_b[:], I["ident_b"], l0, writes=[rc], nowaw=True)
        ph.v("pool", "memset", self.ones_div[:], 1.0 / 1024.0, writes=[rc], nowaw=True)
        ph.v("pool", "memset", self.ones_f[:], 1.0, writes=[rc], nowaw=True)
        rows = self.sb(es, "rows", [128, 3, 128], F32)
        ph.dma("sp", rows[0:8, 0, :], I["c"], l0, writes=[rc], nowaw=True)
        ph.dma("sp", rows[0:96, 1, :], I["mod_b"], l0, writes=[rc], nowaw=True)
        ph.dma("sp", rows[0:64, 2, :], I["norm_g"], l0, writes=[rc], nowaw=True)
        psT = self.ps(es, "psT")
        psM = self.ps(es, "psM")
        rT = Res()
        ph.tr(psT[:, 0:8], rows[0:8, 0, :], self.ident_f[0:8, 0:8], reads=[rc], writes=[rT], )
        ph.tr(psT[:, 128:224], rows[0:96, 1, :], self.ident_f[0:96, 0:96], reads=[rc], writes=[rT])
        ph.tr(psT[:, 256:320], rows[0:64, 2, :], self.ident_f[0:64, 0:64], reads=[rc], writes=[rT])
        small = self.sb(es, "small", [128, 256], F32)
        rs = Res()
        ph.act(small[:, 0:8], psT[:, 0:8], AF.Silu, reads=[rT], writes=[rs])
        ph.v("dve", "tensor_copy", small[:, 8:104], psT[:, 128:224], reads=[rT], writes=[rs], )
        ph.v("dve", "tensor_copy", small[:, 104:168], psT[:, 256:320], reads=[rT], writes=[rs])
        mwp = Pool2([self.sb(es, "mw%d" % i, [128, 8, 1536], F32) for i in range(2)])
        rM = Res()
        for l in range(2):
            for ng in range(4):
                mw, rmw = mwp.next()
                lane = lw[(l * 4 + ng) % 2]
                for c in range(8):
                    ph.dma("sp" if c % 2 == 0 else "act", mw[:, c, :],
                           I["mod_w"][l, c * 128:(c + 1) * 128, ng * 1536:(ng + 1) * 1536], lane,
                           writes=[rmw], nowaw=(c > 0))
                for j in range(12):
                    col = l * 48 + ng * 12 + j
                    for c in range(8):
                        ph.mm(psM[:, col:col + 1], mw[:, c, j * 128:(j + 1) * 128], small[:, c:c + 1],
                              c == 0, c == 7, reads=[rmw, rs], writes=[rM])
        mods = small[:, 168:264] if False else None
        modsT = self.sb(es, "modsT", [128, 96], F32)
        rm = Res()
        ph.v("dve", "tensor_tensor", modsT[:], psM[:, 0:96], small[:, 8:104], ALU.add, reads=[rM, rs], writes=[rm])
        rv = Res()
        for l in range(2):
            b = l * 48
            g = lambda i: small[:, 104 + l * 32 + i * 8: 104 + l * 32 + i * 8 + 8]
            m = lambda i: modsT[:, b + i * 8: b + i * 8 + 8]
            V = self.vecs
            ph.v("dve", "scalar_tensor_tensor", V[:, l, 0, :], m(1), 1.0, g(0), ALU.add, ALU.mult, reads=[rm, rs], writes=[rv], )
            ph.v("dve", "tensor_copy", V[:, l, 1, :], m(0), reads=[rm], writes=[rv])
            ph.v("dve", "tensor_tensor", V[:, l, 2, :], m(2), g(1), ALU.mult, reads=[rm, rs], writes=[rv])
            ph.v("dve", "scalar_tensor_tensor", V[:, l, 3, :], m(4), 1.0, g(2), ALU.add, ALU.mult, reads=[rm, rs], writes=[rv])
            ph.v("dve", "tensor_copy", V[:, l, 4, :], m(3), reads=[rm], writes=[rv])
            ph.v("dve", "tensor_tensor", V[:, l, 5, :], m(5), g(3), ALU.mult, reads=[rm, rs], writes=[rv])
        if self.debug:
            dbgv = self.dscr("dbg_vecs", [128, 96], F32, dbg=True)
            ph.dma("sp", dbgv, self.vecs[:].rearrange("p a b c -> p (a b c)"), l0, reads=[rv])
        ph.emit()
    with ExitStack() as es:
        ph = Phase(K, "prep_b")
        xp = Pool2([self.sb(es, "xt%d" % i, [128, D], F32) for i in range(3)])
        xl = [K.lane() for _ in range(3)]
        hp = Pool2([self.sb(es, "hts%d" % i, [128, 8, 512], F32) for i in range(2)])
        hl = [K.lane() for _ in range(2)]
        banks = [self.ps(es, "pb%d" % i) for i in range(8)]
        rb = [Res() for _ in range(8)]
        hv = Dm["hT0"].rearrange("(c p) t -> p c t", p=128)
        k = 0
        pend_store = []
        for t in range(NT):
            xs = []
            for sub in range(4):
                xt, rx = xp.next()
                r0 = t * 512 + sub * 128
                ph.dma("sp", xt[:], I["x"][r0:r0 + 128, :], xl[k % 3], writes=[rx])
                k += 1
                for c in range(8):
                    ph.tr(banks[c][:, sub * 128:(sub + 1) * 128], xt[:, c * 128:(c + 1) * 128], self.ident_f[:],
                          reads=[rx], writes=[rb[c]])
            hts, rh = hp.next()
            for c in range(8):
                if c % 2 == 0:
                    ph.act(hts[:, c, :], banks[c][:], AF.Copy, reads=[rb[c]], writes=[rh])
                else:
                    ph.v("dve", "tensor_copy", hts[:, c, :], banks[c][:], reads=[rb[c]], writes=[rh])
            pend_store.append((t, hts, rh))
            if len(pend_store) > 1:
                t_, hts_, rh_ = pend_store.pop(0)
                ph.dma("sp", hv[:, :, t_ * 512:(t_ + 1) * 512], hts_[:], hl[t_ % 2], reads=[rh_])
        for t_, hts_, rh_ in pend_store:
            ph.dma("sp", hv[:, :, t_ * 512:(t_ + 1) * 512], hts_[:], hl[t_ % 2], reads=[rh_])
        ph.emit()
    with ExitStack() as es:
        ph = Phase(K, "prep_c")
        l0 = K.lane()
        tab = self.sb(es, "tab", [33, 17], F32)
        rt = Res()
        ph.v("dve", "memset", tab[:], 0.0, writes=[rt])
        ph.v("dve", "memset", tab[32:33, :], -10000.0, writes=[rt])
        ph.dma("sp", tab[0:32, 0:16], I["rbt"], l0, writes=[rt])
        ohc = self.sb(es, "ohc", [33, LC], F32)
        ohd = self.sb(es, "ohd", [33, 3, LD + 1], F32)
        ro = Res()
        ph.dma("sp", ohc[:], I["oh_c"], l0, writes=[ro], nowaw=True)
        ph.dma("sp", ohd[:], I["oh_d"].rearrange("g b m -> b g m"), l0, writes=[ro], nowaw=True)
        lbp = Pool2([self.sb(es, "lb%d" % i, [33, 128], F32) for i in range(2)])
        gp = Pool2([self.sb(es, "gsb%d" % i, [128, LC], BF16) for i in range(2)])
        gl = [K.lane(), K.lane()]
        gdp = Pool2([self.sb(es, "gdb%d" % i, [128, 3, LD + 1], BF16) for i in range(2)])
        gdl = [K.lane(), K.lane()]
        pg = Pool2([self.ps(es, "pg%d" % i) for i in range(4)])
        for s in range(9):
            slot = 8 + s
            lb, rl = lbp.next()
            ph.v("dve", "tensor_copy", lb[:], tab[:, slot:slot + 1].to_broadcast([33, 128]), reads=[rt], writes=[rl])
            gsb, rg = gp.next()
            for n0 in range(0, LC, 512):
                w = min(512, LC - n0)
                pb, rp = pg.next()
                ph.mm(pb[:, 0:w], lb[:], ohc[:, n0:n0 + w], True, True, reads=[rl, ro], writes=[rp])
                ph.act(gsb[:, n0:n0 + w], pb[:, 0:w], AF.Exp, reads=[rp], writes=[rg], )
            ph.dma("sp", Dm["gc"][s], gsb[:], gl[s % 2], reads=[rg])
        for h in range(16):
            lb, rl = lbp.next()
            ph.v("dve", "tensor_copy", lb[:], tab[:, h:h + 1].to_broadcast([33, 128]), reads=[rt], writes=[rl])
            gdb, rg = gdp.next()
            for g in range(3):
                pb, rp = pg.next()
                ph.mm(pb[:, 0:LD + 1], lb[:], ohd[:, g, :], True, True, reads=[rl, ro], writes=[rp])
                ph.act(gdb[:, g, :], pb[:, 0:LD + 1], AF.Exp, reads=[rp], writes=[rg])
            ph.dma("sp", Dm["gd"][h].rearrange("g p m -> p g m"), gdb[:], gdl[h % 2], reads=[rg])
        ph.emit()


Prog.ph_prep = ph_prep


def norm_stats(self, ph, src, rsrc, wk, width):
    sq, rsq = wk["sq"].next()
    ph.act(sq[:, :, 0:width], src, AF.Square, reads=[rsrc], writes=[rsq])
    pb, rp = wk["psn"].next()
    for c in range(8):
        ph.mm(pb[:, 0:width], self.ones_div[:], sq[:, c, 0:width], c == 0, c == 7, reads=[rsq], writes=[rp])
    sd, rsd = wk["sd"].next()
    ph.act(sd[:, 0:width], pb[:, 0:width], AF.Sqrt, reads=[rp, wk["reps"]], writes=[rsd], bias=wk["eps"][:, 0:1])
    rstd, rr = wk["rstd"].next()
    ph.v("dve", "reciprocal", rstd[:, 0:width], sd[:, 0:width], reads=[rsd], writes=[rr])
    return rstd, rr


def norm_modulate(self, ph, ht, rh, layer, which, out, rout, wk, width=512):
    rstd, rr = self.norm_stats(ph, ht, rh, wk, width)
    tt, rt = wk["tt"].next()
    ph.v("dve", "tensor_tensor", tt[:, :, 0:width], ht, rstd[:, None, 0:width].to_broadcast([128, 8, width]), ALU.mult,
         reads=[rh, rr], writes=[rt])
    gi = 0 if which == 1 else 3
    for c in range(8):
        gs = self.vecs[:, layer, gi, c:c + 1]
        sh = self.vecs[:, layer, gi + 1, c:c + 1]
        if c % 2 == 0:
            ph.add("act", lambda e, c=c, gs=gs, sh=sh: e.activation(out=out[:, c, :], in_=tt[:, c, 0:width], func=AF.Identity,
                                                                    bias=sh, scale=gs), [rt], [rout], nowaw=True)
        else:
            ph.add("pool", lambda e, c=c, gs=gs, sh=sh: e.tensor_scalar(out[:, c, :], tt[:, c, 0:width], gs, sh, ALU.mult, ALU.add),
                   [rt], [rout], nowaw=True)


def post_norm_residual(self, ph, yT, ry, ht, rh, layer, which, out, rout, wk, width=512):
    rstd, rr = self.norm_stats(ph, yT, ry, wk, width)
    tt, rt = wk["tt"].next()
    ph.v("dve", "tensor_tensor", tt[:, :, 0:width], yT, rstd[:, None, 0:width].to_broadcast([128, 8, width]), ALU.mult,
         reads=[ry, rr], writes=[rt])
    gi = 2 if which == 1 else 5
    for c in range(8):
        gg = self.vecs[:, layer, gi, c:c + 1]
        eng = "dve"
        ph.add(eng, lambda e, c=c, gg=gg: e.scalar_tensor_tensor(out[:, c, :], tt[:, c, 0:width], gg, ht[:, c, :], ALU.mult, ALU.add),
               [rt, rh], [rout], nowaw=True)


def make_wk(self, es, ph, width=512, nbuf=1):
    wk = {}
    wk["sq"] = Pool2([self.sb(es, "wk_sq%d" % i, [128, 8, width], BF16) for i in range(nbuf)])
    wk["tt"] = Pool2([self.sb(es, "wk_tt%d" % i, [128, 8, width], F32) for i in range(nbuf)])
    wk["sd"] = Pool2([self.sb(es, "wk_sd%d" % i, [128, width], F32) for i in range(2)])
    wk["rstd"] = Pool2([self.sb(es, "wk_rs%d" % i, [128, width], F32) for i in range(2)])
    wk["psn"] = Pool2([self.ps(es, "wk_ps%d" % i) for i in range(2)])
    wk["eps"] = self.sb(es, "wk_eps", [128, 1], F32)
    wk["reps"] = Res()
    ph.v("pool", "memset", wk["eps"][:], EPS, writes=[wk["reps"]])
    return wk


def load_weight_bf16(self, ph, es, name, src, K_rows, N, lane, colstep=1024):
    kc = K_rows // 128
    W = self.sb(es, name, [128, kc, N], BF16)
    rW = Res()
    first = True
    for c in range(kc):
        for n0 in range(0, N, colstep):
            w = min(colstep, N - n0)
            ph.dma("pool", W[:, c, n0:n0 + w], src[c * 128:(c + 1) * 128, n0:n0 + w], lane, writes=[rW], nowaw=not first)
            first = False
    return W, rW


Prog.norm_stats = norm_stats
Prog.norm_modulate = norm_modulate
Prog.post_norm_residual = post_norm_residual
Prog.make_wk = make_wk
Prog.load_weight_bf16 = load_weight_bf16


def ph_inproj(self, layer):
    nc, K, I, Dm = self.nc, self.K, self.I, self.Dm
    hsrc = Dm["hT0"] if layer == 0 else Dm["hT2"]
    hv = hsrc.rearrange("(c p) t -> p c t", p=128)
    with ExitStack() as es:
        ph = Phase(K, "inproj%d" % layer)
        wl = K.lane()
        NW = 3080 if layer == 0 else 3072
        W, rW = self.load_weight_bf16(ph, es, "Win", I["in_w0"] if layer == 0 else I["in_w1"], D, NW, wl)
        wk = self.make_wk(es, ph)
        uT = self.sb(es, "uTall", [128, 8, S], BF16)
        ru = [Res() for _ in range(NT)]
        htp = Pool2([self.sb(es, "ht0", [128, 8, 512], F32)])
        hl = K.lane()
        for t in range(NT):
            ht, rh = htp.next()
            ph.dma("sp", ht[:], hv[:, :, t * 512:(t + 1) * 512], hl, writes=[rh])
            self.norm_modulate(ph, ht[:], rh, layer, 1, uT[:, :, t * 512:(t + 1) * 512], ru[t], wk)
        if self.debug:
            dbu = self.dscr("dbg_uT%d" % layer, [D, S], BF16, dbg=True)
            ph.dma("sp", dbu.rearrange("(c p) t -> p c t", p=128), uT[:], hl, reads=ru)
        if layer == 0:
            qk_cols = [i * 128 for i in range(8)] + [1544 + i * 128 for i in range(8)]
            is_q = [True] * 4 + [False] * 4 + [True] * 4 + [False] * 4
            v_cols = [1024, 2568]
        else:
            qk_cols = [i * 128 for i in range(16)]
            is_q = [True] * 8 + [False] * 8
            v_cols = [2048, 2560]
        pq = Pool2([self.ps(es, "pq%d" % i) for i in range(3)])
        qsb = Pool2([self.sb(es, "qsb%d" % i, [128, 512], BF16) for i in range(3)])
        ql = [K.lane() for _ in range(3)]
        k = 0
        for t in range(NT):
            for oc in range(16):
                pb, rp = pq.next()
                for c in range(8):
                    ph.mm(pb[:], W[:, c, qk_cols[oc]:qk_cols[oc] + 128], uT[:, c, t * 512:(t + 1) * 512], c == 0, c == 7,
                          reads=[rW, ru[t]], writes=[rp])
                qs, rq = qsb.next()
                sc = 0.125 if is_q[oc] else 1.0
                if k % 2 == 0:
                    ph.act(qs[:], pb[:], AF.Copy, reads=[rp], writes=[rq], scale=sc)
                else:
                    ph.v("dve", "tensor_scalar", qs[:], pb[:], sc, None, ALU.mult, reads=[rp], writes=[rq])
                ph.dma("sp", Dm["qkT"][oc * 128:(oc + 1) * 128, t * 512:(t + 1) * 512], qs[:], ql[k % 3], reads=[rq])
                k += 1
        if layer == 0:
            fg = self.sb(es, "fgb", [8, 2], F32)
            rfg = Res()
            ph.dma("sp", fg[:, 0:1], I["fgb"], hl, writes=[rfg])
            ph.v("dve", "tensor_scalar", fg[:, 1:2], fg[:, 0:1], -1.0, None, ALU.mult, reads=[rfg], writes=[rfg])
            pf = self.ps(es, "pf")
            rpf = Res()
            fsb = Pool2([self.sb(es, "fsb%d" % i, [8, 2, 512], F32) for i in range(2)])
            fl = [K.lane(), K.lane()]
            for t in range(NT):
                for c in range(8):
                    ph.mm(pf[0:8, :], W[:, c, 1536:1544], uT[:, c, t * 512:(t + 1) * 512], c == 0, c == 7,
                          reads=[rW, ru[t]], writes=[rpf])
                fs, rf = fsb.next()
                ph.act(fs[:, 0, :], pf[0:8, :], AF.Exp, reads=[rpf, rfg], writes=[rf], bias=fg[:, 1:2], scale=-1.0)
                ph.act(fs[:, 1, :], fs[:, 0, :], AF.Ln, reads=[rf], writes=[rf], bias=1.0)
                ph.dma("sp", self.spT[:, t * 512:(t + 1) * 512], fs[:, 1, :], fl[t % 2], reads=[rf])
        pv = Pool2([self.ps(es, "pv%d" % i) for i in range(2)])
        vtp = Pool2([self.sb(es, "vt%d" % i, [128, 16, 4, 65], BF16) for i in range(2)])
        vl = [K.lane(), K.lane()]
        for vt_, rv_ in zip(vtp.tiles, vtp.res):
            ph.v("pool", "memset", vt_[:], 1.0, writes=[rv_])
        pats = [1] if layer == 0 else list(DILS)
        k = 0
        for gi, dil in enumerate(pats):
            Ls = S // dil
            for grp in range(8):
                vt, rv = vtp.next()
                for b4 in range(4):
                    m0 = (grp * 4 + b4) * 128
                    r, n0 = m0 // Ls, m0 % Ls
                    t0 = n0 * dil + r
                    tiles_touched = sorted(set([(t0 + j * dil) // 512 for j in (0, 127)]))
                    tiles_touched = list(range(tiles_touched[0], tiles_touched[-1] + 1))
                    for half in range(2):
                        pb, rp = pv.next()
                        for c in range(8):
                            ph.mm(pb[:], uT[:, c, t0:t0 + 127 * dil + 1:dil], W[:, c, v_cols[half]:v_cols[half] + 512],
                                  c == 0, c == 7, reads=[rW] + [ru[x] for x in tiles_touched], writes=[rp])
                        src = pb[:].rearrange("p (h e) -> p h e", e=64)
                        dst = vt[:, half * 8:(half + 1) * 8, b4, 0:64]
                        if k % 2 == 0:
                            ph.act(dst, src, AF.Copy, reads=[rp], writes=[rv], )
                        else:
                            ph.v("dve", "tensor_copy", dst, src, reads=[rp], writes=[rv])
                        k += 1
                dv = Dm["vaug"][gi][:, :, grp * 260:(grp + 1) * 260].rearrange("h p f -> p h f")
                ph.dma("sp", dv, vt[:].rearrange("p h b e -> p h (b e)"), vl[grp % 2], reads=[rv])
        ph.emit()


Prog.ph_inproj = ph_inproj


def ph_cum(self):
    nc, K, I, Dm = self.nc, self.K, self.I, self.Dm
    CH = 1024
    with ExitStack() as es:
        ph = Phase(K, "cum")
        l0, l1 = K.lane(), K.lane()
        ones = self.sb(es, "c_ones", [8, CH], F32)
        r1 = Res()
        ph.v("dve", "memset", ones[:], 1.0, writes=[r1])
        spp = Pool2([self.sb(es, "c_sp%d" % i, [8, CH], F32) for i in range(2)])
        Sp = Pool2([self.sb(es, "c_S%d" % i, [8, CH], F32) for i in range(2)])
        cpp = Pool2([self.sb(es, "c_cp%d" % i, [8, 6, CH], BF16) for i in range(2)])
        ra = self.sb(es, "c_ra", [8, CH], F32)
        rb = self.sb(es, "c_rb", [8, CH], F32)
        rr = Res()
        prevS = None
        for ci in range(S // CH):
            sp, rs = spp.next()
            ph.dma("sp", sp[:], self.spT[:, ci * CH:(ci + 1) * CH], l0, writes=[rs])
            Sc, rS = Sp.next()
            init = 0.0 if prevS is None else prevS[0][:, CH - 1:CH]
            rd = [rs, r1] + ([] if prevS is None else [prevS[1]])
            ph.v("dve", "tensor_tensor_scan", Sc[:], ones[:], sp[:], init, ALU.mult, ALU.add, reads=rd, writes=[rS])
            prevS = (Sc, rS)
            cp, rc = cpp.next()
            ph.v("dve", "tensor_copy", cp[:, 0, :], Sc[:], reads=[rS], writes=[rc])
            ph.v("dve", "tensor_tensor", ra[:], Sc[:], cp[:, 0, :], ALU.subtract, reads=[rS, rc], writes=[rr])
            ph.v("dve", "tensor_copy", cp[:, 1, :], ra[:], reads=[rr], writes=[rc])
            ph.v("dve", "tensor_tensor", rb[:], ra[:], cp[:, 1, :], ALU.subtract, reads=[rr, rc], writes=[rr])
            ph.v("dve", "tensor_copy", cp[:, 2, :], rb[:], reads=[rr], writes=[rc])
            ph.v("dve", "tensor_scalar", cp[:, 3:6, :], cp[:, 0:3, :], -1.0, None, ALU.mult, reads=[rc], writes=[rc])
            ph.dma("sp", Dm["cparts"][:, :, ci * CH:(ci + 1) * CH], cp[:], l1, reads=[rc])
        ph.emit()


Prog.ph_cum = ph_cum


def _in_maps(inputs, cores):
    c = make_consts()
    f = lambda a: np.ascontiguousarray(np.asarray(a, dtype=np.float32))
    shared = {
        "mod_w": f(inputs["mod_w"]),
        "mod_b": f(inputs["mod_b"]).reshape(96, 128),
        "norm_g": f(inputs["norm_g"]).reshape(64, 128),
        "in_w0": f(inputs["attn_in_w_even"][0]),
        "fgb": f(inputs["fox_gate_bias"][0]).reshape(8, 1),
        "out_w0": f(inputs["attn_out_w_even"][0]),
        "in_w1": f(inputs["attn_in_w_odd"][0]),
        "out_w1": f(inputs["attn_out_w_odd"][0]),
        "rbt": f(inputs["rel_bias_table"]),
        "ffn_g": f(inputs["ffn_w_gate"]),
        "ffn_u": f(inputs["ffn_w_up"]),
        "ffn_d": f(inputs["ffn_w_down"]),
        "router": f(inputs["router_w"][0]),
        "exp_g": f(inputs["exp_w_gate"][0]),
        "exp_u": f(inputs["exp_w_up"][0]),
        "exp_d": f(inputs["exp_w_down"][0]),
    }
    shared.update(c)
    maps = []
    for b in cores:
        m = dict(shared)
        m["x"] = f(inputs["x"][b])
        m["c"] = f(inputs["c"][b]).reshape(8, 128)
        maps.append(m)
    return maps


def kernel(**inputs):
    prog = Prog()
    nc = prog.build()
    maps = _in_maps(inputs, list(range(8)))
    res = run_bass_kernel_spmd(nc, maps, core_ids=list(range(8)))
    return np.stack([np.asarray(r["out"], dtype=np.float32) for r in res.results], axis=0)


def ph_attn01(self, moba):
    nc, K, I, Dm = self.nc, self.K, self.I, self.Dm
    KD = 80 if moba else 70
    gc = Dm["gc"]
    with ExitStack() as es:
        ph = Phase(K, "moba" if moba else "fox")
        Qa = [self.sb(es, "Qa%d" % i, [128, S], BF16) for i in range(2)]
        Ka = [self.sb(es, "Ka%d" % i, [128, S], BF16) for i in range(2)]
        Vg = [self.sb(es, "Vg%d" % i, [128, 32 * 65 + 64], BF16) for i in range(2)]
        rQ = [Res(), Res()]
        rK = [Res(), Res()]
        rV = [Res(), Res()]
        for b in range(2):
            ph.v("pool", "memset", Qa[b][64:128, :], 0.0, writes=[rQ[b]])
            ph.v("pool", "memset", Ka[b][64:128, :], 0.0, writes=[rK[b]])
            ph.v("pool", "memset", Vg[b][:, 2080:2144], 0.0, writes=[rV[b]])
        lq = [K.lane(), K.lane()]
        lk = [K.lane(), K.lane()]
        lv = [K.lane(), K.lane()]
        lo = [K.lane(), K.lane()]
        Oacc = [self.sb(es, "Oacc%d" % i, [128, S], F32) for i in range(2)]
        rOa = [Res(), Res()]
        ptp = Pool2([self.sb(es, "Pt%d" % i, [128, 512], BF16) for i in range(8)])
        psS = Pool2([self.ps(es, "psS%d" % i) for i in range(3 if moba else 4)])
        psO = Pool2([self.ps(es, "psO%d" % i) for i in range(2)])
        psL = Pool2([self.ps(es, "psL%d" % i) for i in range(1)])
        rinvp = Pool2([self.sb(es, "rinv%d" % i, [128, 512], F32) for i in range(2)])
        osbp = Pool2([self.sb(es, "osb%d" % i, [128, 512], BF16) for i in range(2)])
        if moba:
            Th = [self.sb(es, "Th%d" % i, [128, WC], BF16) for i in range(2)]
            rT = [Res(), Res()]
            lt = [K.lane(), K.lane()]
            for b in range(2):
                ph.dma("sp", Ka[b][64:80, :], I["ind"], lk[b], writes=[rK[b]])
            psG = self.ps(es, "psG")
            rG = Res()
            psT = self.ps(es, "psT")
            rPT = Res()
            ksf = self.sb(es, "ksf", [128, 16], F32)
            ksb = self.sb(es, "ksb", [128, 16], BF16)
            rks = Res()
            gsb = self.sb(es, "gsb", [128, 16], F32)
            mx8 = self.sb(es, "mx8", [128, 8], F32)
            selT = self.sb(es, "selT", [128, 80], BF16)
            rg = Res()
            rsel = Res()
            ph.v("dve", "memset", selT[:], 0.0, writes=[rsel])
        else:
            T0 = self.sb(es, "T0", [128, WC], BF16)
            rT0 = Res()
            lt0 = K.lane()
            ph.dma("sp", T0[:], AP(gc.tensor, 8 * 128 * LC + 127, [[LC - 1, 128], [1, WC]]), lt0, writes=[rT0])
            tmpp = Pool2([self.sb(es, "tmp%d" % i, [128, 512], F32) for i in range(2)])
            for b in range(2):
                ph.v("pool", "memset", Qa[b][64:70, :], 1.0, writes=[rQ[b]])
                ph.v("pool", "memset", Ka[b][64:70, :], 1.0, writes=[rK[b]])
        kk = 0
        LA = 6

        def prep_loads(h):
            b = h % 2
            qrow = (1024 if moba else 0) + h * 64
            krow = (1536 if moba else 512) + h * 64
            hg = (8 + h) if moba else h
            ph.dma("sp", Qa[b][0:64, :], Dm["qkT"][qrow:qrow + 64, :], lq[b], writes=[rQ[b]])
            ph.dma("sp", Ka[b][0:64, :], Dm["qkT"][krow:krow + 64, :], lk[b], writes=[rK[b]])
            ph.dma("sp", Vg[b][:, 0:2080], Dm["vaug"][0][hg], lv[b], writes=[rV[b]])
            if moba:
                ph.dma("sp", Th[b][:], AP(gc.tensor, h * 128 * LC + 127, [[LC - 1, 128], [1, WC]]), lt[b], writes=[rT[b]])
            else:
                ph.dma("sp", Qa[b][64:67, :], Dm["cparts"][h, 3:6, :], lq[b], writes=[rQ[b]], nowaw=True)
                ph.dma("sp", Ka[b][67:70, :], Dm["cparts"][h, 0:3, :], lk[b], writes=[rK[b]], nowaw=True)

        def gate_chunks(h):
            b = h % 2
            ch = []
            if not moba:
                return ch

            def c0():
                ph.v("dve", "tensor_reduce", ksf[0:64, :], Ka[b][0:64, :].rearrange("p (j s) -> p j s", s=256), AX.X, ALU.add,
                     reads=[rK[b]], writes=[rks])
                ph.v("dve", "tensor_copy", ksb[0:64, :], ksf[0:64, :], reads=[rks], writes=[rks])
                ph.v("dve", "memset", gsb[:], -1e30, writes=[rg])
                ph.v("dve", "memset", selT[:, 64:80], NEG, writes=[rsel])
            ch.append(c0)
            for tt in range(32):
                def cA(tt=tt):
                    own = tt // 2
                    if tt % 2 == 0:
                        ph.v("dve", "memset", selT[:, 64 + own:65 + own], 0.0, writes=[rsel])
                    if own >= 3:
                        ph.mm(psG[:, 0:16], Qa[b][0:64, tt * 128:(tt + 1) * 128], ksb[0:64, 0:16], True, True,
                              reads=[rQ[b], rks], writes=[rG])
                        ph.v("dve", "tensor_copy", gsb[:, 0:own], psG[:, 0:own], reads=[rG], writes=[rg])
                        ph.v("dve", "max", mx8[:], gsb[:, 0:16], reads=[rg], writes=[rg])
                        ph.v("dve", "tensor_scalar", selT[:, 64:64 + own], gsb[:, 0:own], mx8[:, 2:3], NEG, ALU.is_lt, ALU.mult,
                             reads=[rg], writes=[rsel])

                def cC(tt=tt):
                    ph.mm(psT[0:80, 0:128], selT[:, 0:80], self.ident_b[:], True, True, reads=[rsel], writes=[rPT])
                    ph.add("act", lambda e, tt=tt: e.activation(out=Qa[b][64:80, tt * 128:(tt + 1) * 128], in_=psT[64:80, 0:128],
                                                                func=AF.Copy), [rPT], [rQ[b]], nowaw=True)
                ch.append(cA)
                ch.append(cC)
            return ch

        def sweep(h, chunks):
            nonlocal kk
            b = h % 2
            hg = (8 + h) if moba else h
            items = [(qt, kb) for qt in range(NT) for kb in range(4 * (qt + 1))]
            st = {}

            def stage1(it):
                nonlocal kk
                qt, kb = it
                pS, rS = psS.next()
                ph.mm(pS[:], Ka[b][:, kb * 128:(kb + 1) * 128], Qa[b][:, qt * 512:(qt + 1) * 512], True, True,
                      reads=[rK[b], rQ[b]], writes=[rS])
                Pt, rP = ptp.next()
                delta = 512 * qt - 128 * kb
                if moba:
                    ph.act(Pt[:], pS[:], AF.Exp, reads=[rS], writes=[rP])
                    a = min(delta, 2304) + 384
                    ph.v("pool" if kk % 4 == 3 else "dve", "tensor_tensor", Pt[:], Pt[:], Th[b][:, a:a + 512], ALU.mult,
                         reads=[rP, rT[b]], writes=[rP])
                    kk += 1
                elif delta <= 0:
                    tmp, rtm = tmpp.next()
                    ph.v("dve", "tensor_scalar", tmp[:], pS[:], 60.0, None, ALU.min, reads=[rS], writes=[rtm])
                    ph.act(Pt[:], tmp[:], AF.Exp, reads=[rtm], writes=[rP])
                    a = delta + 384
                    ph.v("dve", "tensor_tensor", Pt[:], Pt[:], T0[:, a:a + 512], ALU.mult, reads=[rP, rT0], writes=[rP])
                else:
                    ph.act(Pt[:], pS[:], AF.Exp, reads=[rS], writes=[rP])
                st[it] = (Pt, rP)

            def stage2(it):
                qt, kb = it
                nkb = 4 * (qt + 1)
                if kb == 0:
                    st["O"] = psO.next()
                pO, rO = st["O"]
                Pt, rP = st.pop(it)
                ph.mm(pO[:, :], Vg[b][:, kb * 65:kb * 65 + 128], Pt[:], kb == 0, kb == nkb - 1, reads=[rV[b], rP], writes=[rO])
                if kb == nkb - 1:
                    self.finish_chunk(ph, pO, rO, Oacc[b], rOa[b], qt, hg, psL, rinvp, osbp, lo, first=(qt == 0))

            for i in range(len(items) + LA):
                if i < len(items):
                    stage1(items[i])
                if i >= LA:
                    stage2(items[i - LA])
                if chunks and i % 2 == 1:
                    chunks.pop(0)()
            while chunks:
                chunks.pop(0)()

        prep_loads(0)
        for c_ in gate_chunks(0):
            c_()
        for h in range(8):
            nxt = []
            if h + 1 < 8:
                prep_loads(h + 1)
                nxt = gate_chunks(h + 1)
            sweep(h, nxt)
        ph.emit()


def finish_chunk(self, ph, pO, rO, Oacc, rOa, qt, hg, psL, rinvp, osbp, lo, first, src_is_sbuf=False):
    cs = slice(qt * 512, (qt + 1) * 512)
    if not src_is_sbuf:
        ph.add("act", lambda e: e.activation(out=Oacc[0:65, cs], in_=pO[0:65, :], func=AF.Copy), [rO], [rOa], nowaw=not first)
    pL, rL = psL.next()
    ph.mm(pL[0:64, :], self.ones_f[64:65, 0:64], Oacc[64:65, cs], True, True, reads=[rOa], writes=[rL])
    rinv, rri = rinvp.next()
    ph.act(rinv[0:64, :], pL[0:64, :], AF.Ln, reads=[rL], writes=[rri])
    ph.act(rinv[0:64, :], rinv[0:64, :], AF.Exp, reads=[rri], writes=[rri], scale=-1.0)
    osb, ros = osbp.next()
    ph.v("pool", "tensor_tensor", osb[0:64, :], Oacc[0:64, cs], rinv[0:64, :], ALU.mult, reads=[rOa, rri], writes=[ros])
    ph.dma("sp", self.Dm["oT"][hg * 64:(hg + 1) * 64, cs], osb[0:64, :], lo[qt % 2], reads=[ros])


Prog.ph_attn01 = ph_attn01
Prog.finish_chunk = finish_chunk


def ph_outproj(self, layer):
    nc, K, I, Dm = self.nc, self.K, self.I, self.Dm
    hsrc = Dm["hT0"] if layer == 0 else Dm["hT2"]
    hdst = Dm["hT1"] if layer == 0 else Dm["hT3"]
    hv = hsrc.rearrange("(c p) t -> p c t", p=128)
    hd = hdst.rearrange("(c p) t -> p c t", p=128)
    ov = Dm["oT"].rearrange("(c p) t -> p c t", p=128)
    with ExitStack() as es:
        ph = Phase(K, "outproj%d" % layer)
        Wo, rW = self.load_weight_bf16(ph, es, "Wo", I["out_w0"] if layer == 0 else I["out_w1"], D, D, K.lane())
        wk = self.make_wk(es, ph)
        otp = Pool2([self.sb(es, "ot%d" % i, [128, 8, 512], BF16) for i in range(2)])
        htp = Pool2([self.sb(es, "ht%d" % i, [128, 8, 512], F32) for i in range(2)])
        ytp = Pool2([self.sb(es, "yt%d" % i, [128, 8, 512], F32) for i in range(2)])
        l1 = [K.lane(), K.lane()]
        l2 = [K.lane(), K.lane()]
        l3 = [K.lane(), K.lane()]
        py = Pool2([self.ps(es, "py%d" % i) for i in range(4)])
        def loads(t):
            ts_ = slice(t * 512, (t + 1) * 512)
            ot, ro = otp.next()
            ph.dma("sp", ot[:], ov[:, :, ts_], l1[t % 2], writes=[ro])
            ht, rh = htp.next()
            ph.dma("sp", ht[:], hv[:, :, ts_], l2[t % 2], writes=[rh])
            return ot, ro, ht, rh

        nxt = loads(0)
        for t in range(NT):
            ts_ = slice(t * 512, (t + 1) * 512)
            ot, ro, ht, rh = nxt
            if t + 1 < NT:
                nxt = loads(t + 1)
            yt, ry = ytp.next()
            for dc in range(8):
                pb, rp = py.next()
                for c in range(8):
                    ph.mm(pb[:], Wo[:, c, dc * 128:(dc + 1) * 128], ot[:, c, :], c == 0, c == 7, reads=[rW, ro], writes=[rp])
                if dc % 2 == 0:
                    ph.add("act", lambda e, yt=yt, pb=pb, dc=dc: e.activation(out=yt[:, dc, :], in_=pb[:], func=AF.Copy), [rp], [ry],
                           nowaw=(dc > 0))
                else:
                    ph.add("dve", lambda e, yt=yt, pb=pb, dc=dc: e.tensor_copy(yt[:, dc, :], pb[:]), [rp], [ry], nowaw=True)
            self.post_norm_residual(ph, yt[:], ry, ht[:], rh, layer, 1, ht[:], rh, wk)
            ph.dma("sp", hd[:, :, ts_], ht[:], l3[t % 2], reads=[rh])
        ph.emit()


Prog.ph_outproj = ph_outproj


def ph_ffn(self, layer):
    nc, K, I, Dm = self.nc, self.K, self.I, self.Dm
    E = 1 if layer == 0 else 8
    F = 2816 if layer == 0 else 3584
    nf = F // 128
    groups = []
    f0 = 0
    while f0 < nf:
        n = min(4, nf - f0)
        groups.append((f0, n))
        f0 += n
    hsrc = Dm["hT1"] if layer == 0 else Dm["hT3"]
    hv = hsrc.rearrange("(c p) t -> p c t", p=128)
    hd = Dm["hT2"].rearrange("(c p) t -> p c t", p=128)
    if layer == 0:
        Wg, Wu, Wd = I["ffn_g"], I["ffn_u"], I["ffn_d"]
    else:
        Wg, Wu, Wd = I["exp_g"], I["exp_u"], I["exp_d"]
    HALF = 2048
    SW = 256
    with ExitStack() as es:
        ph = Phase(K, "ffn%d" % layer)
        wk = self.make_wk(es, ph, width=SW)
        yacc = self.sb(es, "yacc", [128, 8, HALF], F32)
        uT = self.sb(es, "uTh", [128, 8, HALF], BF16)
        wgp = [self.sb(es, "wg%d" % i, [128, 8, 512], BF16) for i in range(2)]
        wup = [self.sb(es, "wu%d" % i, [128, 8, 512], BF16) for i in range(2)]
        wdp = [self.sb(es, "wd%d" % i, [128, 4, D], BF16) for i in range(2)]
        rw = [Res(), Res()]
        lw = [K.lane(), K.lane()]
        hmid = self.sb(es, "hmid", [128, 4, 512], BF16)
        rhm = Res()
        sp_ = Pool2([self.sb(es, "ssb%d" % i, [128, 512], BF16) for i in range(2)])
        htp = Pool2([self.sb(es, "hts", [128, 8, SW], F32)])
        lh = K.lane()
        lst = K.lane()
        pa = Pool2([self.ps(es, "pa%d" % i) for i in range(2)])
        pbk = Pool2([self.ps(es, "pb%d" % i) for i in range(2)])
        pyk = Pool2([self.ps(es, "py%d" % i) for i in range(2)])
        if E > 1:
            bgp = Pool2([self.sb(es, "bg%d" % i, [128, 512], F32) for i in range(2)])
            gbc = self.sb(es, "gbc", [128, HALF], F32)
            rgb = Res()
            gT = self.sb(es, "gT", [8, HALF], F32)
            rgT = Res()
            selE = self.sb(es, "selE", [8, 8, 128], F32)
            rse = Res()
            ph.v("dve", "tensor_copy", selE[:], self.ident_f[0:8, 0:8, None].to_broadcast([8, 8, 128]), writes=[rse])
            Wr, rWr = self.load_weight_bf16(ph, es, "Wr", I["router"], D, 8, K.lane())
            lg = self.sb(es, "lg", [128, 8], F32)
            mx = self.sb(es, "mx", [128, 8], F32)
            pr = self.sb(es, "pr", [128, 4], F32)
            gg = self.sb(es, "gg", [128, 2, 8], F32)
            rl = Res()
            otp = Pool2([self.sb(es, "otile%d" % i, [128, D], F32) for i in range(1)])
            lot = [K.lane(), K.lane()]
        wcount = 0
        for hf in range(S // HALF):
            ru = Res()
            for sub in range(HALF // SW):
                tok = hf * HALF + sub * SW
                ht, rh = htp.next()
                ph.dma("sp", ht[:], hv[:, :, tok:tok + SW], lh, writes=[rh])
                self.norm_modulate(ph, ht[:], rh, layer, 2, uT[:, :, sub * SW:(sub + 1) * SW], ru, wk, width=SW)
                if E > 1:
                    for s2 in range(SW // 128):
                        c0 = sub * SW + s2 * 128
                        pb, rp = pbk.next()
                        for c in range(8):
                            ph.mm(pb[:, 0:8], uT[:, c, c0:c0 + 128], Wr[:, c, 0:8], c == 0, c == 7, reads=[ru, rWr], writes=[rp])
                        ph.v("dve", "tensor_copy", lg[:], pb[:, 0:8], reads=[rp], writes=[rl])
                        ph.v("dve", "max", mx[:], lg[:], reads=[rl], writes=[rl])
                        ph.v("dve", "tensor_tensor", pr[:, 0:1], mx[:, 0:1], mx[:, 1:2], ALU.subtract, reads=[rl], writes=[rl])
                        ph.act(pr[:, 1:2], pr[:, 0:1], AF.Sigmoid, reads=[rl], writes=[rl])
                        ph.act(pr[:, 2:3], pr[:, 0:1], AF.Sigmoid, reads=[rl], writes=[rl], scale=-1.0)
                        ph.v("dve", "tensor_scalar", gg[:, 0, :], lg[:], mx[:, 0:1], pr[:, 1:2], ALU.is_ge, ALU.mult, reads=[rl], writes=[rl])
                        ph.v("dve", "tensor_scalar", gg[:, 1, :], lg[:], mx[:, 1:2], pr[:, 2:3], ALU.is_equal, ALU.mult, reads=[rl], writes=[rl])
                        ph.v("dve", "tensor_tensor", gg[:, 0, :], gg[:, 0, :], gg[:, 1, :], ALU.add, reads=[rl], writes=[rl])
                        pb2, rp2 = pbk.next()
                        ph.tr(pb2[0:8, 0:128], gg[:, 0, :], self.ident_f[:], reads=[rl], writes=[rp2])
                        ph.add("act", lambda e, c0=c0, pb2=pb2: e.activation(out=gT[0:8, c0:c0 + 128], in_=pb2[0:8, 0:128], func=AF.Copy),
                               [rp2], [rgT], nowaw=True)
            for e_ in range(E):
                if E > 1:
                    for q in range(HALF // 512):
                        pb, rp = pbk.next()
                        ph.mm(pb[:], selE[0:8, e_, :], gT[0:8, q * 512:(q + 1) * 512], True, True, reads=[rse, rgT], writes=[rp])
                        ph.add("act", lambda e, q=q, pb=pb: e.activation(out=gbc[:, q * 512:(q + 1) * 512], in_=pb[:], func=AF.Copy),
                               [rp], [rgb], nowaw=(q > 0))
                for gi_, (f0, n) in enumerate(groups):
                    wb = wcount % 2
                    wcount += 1
                    wg, wu, wd = wgp[wb], wup[wb], wdp[wb]
                    first = True
                    for c in range(8):
                        ph.dma("pool", wg[:, c, 0:n * 128], Wg[e_, c * 128:(c + 1) * 128, f0 * 128:(f0 + n) * 128], lw[wb],
                               writes=[rw[wb]], nowaw=not first)
                        first = False
                        ph.dma("pool", wu[:, c, 0:n * 128], Wu[e_, c * 128:(c + 1) * 128, f0 * 128:(f0 + n) * 128], lw[wb],
                               writes=[rw[wb]], nowaw=True)
                    for j in range(n):
                        ph.dma("pool", wd[:, j, :], Wd[e_, (f0 + j) * 128:(f0 + j + 1) * 128, :], lw[wb], writes=[rw[wb]], nowaw=True)
                    for tq in range(HALF // 512):
                        tsl = slice(tq * 512, (tq + 1) * 512)
                        for j in range(n):
                            pA, rA = pa.next()
                            pB, rB = pbk.next()
                            for c in range(8):
                                ph.mm(pA[:], wg[:, c, j * 128:(j + 1) * 128], uT[:, c, tsl], c == 0, c == 7, reads=[rw[wb], ru], writes=[rA])
                            for c in range(8):
                                ph.mm(pB[:], wu[:, c, j * 128:(j + 1) * 128], uT[:, c, tsl], c == 0, c == 7, reads=[rw[wb], ru], writes=[rB])
                            ss, rs = sp_.next()
                            ph.act(ss[:], pA[:], AF.Silu, reads=[rA], writes=[rs])
                            if E > 1:
                                bg, rbg = bgp.next()
                                ph.v("dve", "tensor_tensor", bg[:], pB[:], gbc[:, tsl], ALU.mult, reads=[rB, rgb], writes=[rbg])
                                ph.add("pool", lambda e, j=j, ss=ss, bg=bg: e.tensor_tensor(hmid[:, j, :], ss[:], bg[:], ALU.mult),
                                       [rs, rbg], [rhm], nowaw=(j > 0))
                            else:
                                ph.add("dve", lambda e, j=j, ss=ss, pB=pB: e.tensor_tensor(hmid[:, j, :], ss[:], pB[:], ALU.mult),
                                       [rs, rB], [rhm], nowaw=(j > 0))
                        for dc in range(8):
                            pY, rY = pyk.next()
                            for j in range(n):
                                ph.mm(pY[:], wd[:, j, dc * 128:(dc + 1) * 128], hmid[:, j, :], j == 0, j == n - 1, reads=[rw[wb], rhm], writes=[rY])
                            if e_ == 0 and gi_ == 0:
                                ph.add("act", lambda e, dc=dc, pY=pY, tsl=tsl: e.activation(out=yacc[:, dc, tsl], in_=pY[:], func=AF.Copy),
                                       [rY], [self._ry(tq, dc)])
                            else:
                                ph.add("dve", lambda e, dc=dc, pY=pY, tsl=tsl: e.tensor_tensor(yacc[:, dc, tsl], yacc[:, dc, tsl], pY[:], ALU.add),
                                       [rY, self._ry(tq, dc)], [self._ry(tq, dc)])
            for sub in range(HALF // SW):
                tok = hf * HALF + sub * SW
                ht, rh = htp.next()
                ph.dma("sp", ht[:], hv[:, :, tok:tok + SW], lh, writes=[rh])
                rys = [self._ry((sub * SW) // 512, dc) for dc in range(8)]
                ryall = Res()
                ryall.writers = [w for r in rys for w in r.writers]
                self.post_norm_residual(ph, yacc[:, :, sub * SW:(sub + 1) * SW], ryall, ht[:], rh, layer, 2, ht[:], rh, wk, width=SW)
                for r in rys:
                    r.readers.extend(ryall.readers)
                if layer == 0:
                    ph.dma("sp", hd[:, :, tok:tok + SW], ht[:], lst, reads=[rh])
                else:
                    for s2 in range(SW // 128):
                        ot, rot = otp.next()
                        for hh in range(2):
                            pX, rX = pa.next()
                            for c4 in range(4):
                                c = hh * 4 + c4
                                ph.tr(pX[:, c4 * 128:(c4 + 1) * 128], ht[:, c, s2 * 128:(s2 + 1) * 128], self.ident_f[:], reads=[rh], writes=[rX])
                            if hh == 0:
                                ph.add("act", lambda e, ot=ot, pX=pX: e.activation(out=ot[:, 0:512], in_=pX[:], func=AF.Copy), [rX], [rot])
                            else:
                                ph.add("dve", lambda e, ot=ot, pX=pX: e.tensor_copy(ot[:, 512:1024], pX[:]), [rX], [rot], nowaw=True)
                        ph.dma("sp", self.out[tok + s2 * 128: tok + (s2 + 1) * 128, :], ot[:], lot[s2 % 2], reads=[rot])
            self._rycache = {}
        ph.emit()


def _ry(self, tq, dc):
    if not hasattr(self, "_rycache"):
        self._rycache = {}
    key = (tq, dc)
    if key not in self._rycache:
        self._rycache[key] = Res()
    return self._rycache[key]


Prog.ph_ffn = ph_ffn
Prog._ry = _ry


def ph_dil(self):
    nc, K, I, Dm = self.nc, self.K, self.I, self.Dm
    gd = Dm["gd"]
    with ExitStack() as es:
        ph = Phase(K, "dil")
        Qn = [self.sb(es, "Qn%d" % i, [128, S], BF16) for i in range(2)]
        Kn = [self.sb(es, "Kn%d" % i, [128, S], BF16) for i in range(2)]
        Qd = [[self.sb(es, "Qd%d_%d" % (i, g), [128, S], BF16) for g in range(2)] for i in range(2)]
        Kd = [[self.sb(es, "Kd%d_%d" % (i, g), [128, S], BF16) for g in range(2)] for i in range(2)]
        Vd = [[self.sb(es, "Vd%d_%d" % (i, g), [128, 32 * 65 + 64], BF16) for g in range(3)] for i in range(2)]
        Td = [self.sb(es, "Td%d" % i, [128, 3, 256], BF16) for i in range(2)]
        rQ, rK, rV, rT = [Res(), Res()], [Res(), Res()], [Res(), Res()], [Res(), Res()]
        rQd = [[Res(), Res()], [Res(), Res()]]
        rKd = [[Res(), Res()], [Res(), Res()]]
        lq, lk, lv, lt, lo = ([K.lane(), K.lane()] for _ in range(5))
        Oacc = [self.sb(es, "Oacc%d" % i, [128, S], F32) for i in range(2)]
        rOa = [Res(), Res()]
        ptp = Pool2([self.sb(es, "Pt%d" % i, [128, 256], BF16) for i in range(8)])
        psS = Pool2([self.ps(es, "psS%d" % i) for i in range(4)])
        obanks = [self.ps(es, "psO%d" % i) for i in range(3)]
        rOb = [Res() for _ in range(3)]
        psL = Pool2([self.ps(es, "psL0")])
        rinvp = Pool2([self.sb(es, "rinv%d" % i, [128, 512], F32) for i in range(2)])
        osbp = Pool2([self.sb(es, "osb%d" % i, [128, 512], BF16) for i in range(2)])
        for b in range(2):
            ph.v("pool", "memset", Qn[b][64:128, :], 0.0, writes=[rQ[b]])
            ph.v("pool", "memset", Kn[b][64:128, :], 0.0, writes=[rK[b]])
            for g in range(2):
                ph.v("pool", "memset", Qd[b][g][64:128, :], 0.0, writes=[rQd[b][g]])
                ph.v("pool", "memset", Kd[b][g][64:128, :], 0.0, writes=[rKd[b][g]])
            for g in range(3):
                ph.v("pool", "memset", Vd[b][g][:, 2080:2144], 0.0, writes=[rV[b]], nowaw=(g > 0))
        kk = 0
        LA = 6

        def prep_loads(h):
            b = h % 2
            ph.dma("sp", Qn[b][0:64, :], Dm["qkT"][h * 64:(h + 1) * 64, :], lq[b], writes=[rQ[b]])
            ph.dma("sp", Kn[b][0:64, :], Dm["qkT"][1024 + h * 64:1024 + (h + 1) * 64, :], lk[b], writes=[rK[b]])
            for g in range(3):
                ph.dma("sp", Vd[b][g][:, 0:2080], Dm["vaug"][g][h], lv[b], writes=[rV[b]], nowaw=(g > 0))
            ph.dma("sp", Td[b][:], AP(gd.tensor, h * 3 * 128 * (LD + 1) + 127, [[LD, 128], [128 * (LD + 1), 3], [1, 256]]), lt[b],
                   writes=[rT[b]])

        def prep_chunks(h):
            b = h % 2
            ch = []
            for g in (1, 2):
                dil = DILS[g]

                def cq(g=g, dil=dil):
                    ph.v("dve", "tensor_copy", Qd[b][g - 1][0:64, :].rearrange("p (r n) -> p r n", r=dil),
                         Qn[b][0:64, :].rearrange("p (n r) -> p r n", r=dil), reads=[rQ[b]], writes=[rQd[b][g - 1]])

                def ck(g=g, dil=dil):
                    ph.add("act", lambda e: e.activation(out=Kd[b][g - 1][0:64, :].rearrange("p (r n) -> p r n", r=dil),
                                                         in_=Kn[b][0:64, :].rearrange("p (n r) -> p r n", r=dil), func=AF.Copy),
                           [rK[b]], [rKd[b][g - 1]])
                ch.append(cq)
                ch.append(ck)
            return ch

        def sweep(h, chunks):
            nonlocal kk
            b = h % 2
            items = []
            nbank = 0
            for g in range(3):
                dil = DILS[g]
                nb = (S // dil) // 128
                for r in range(dil):
                    for kb in range(nb):
                        items.append((g, r, kb, nbank))
                nbank += (dil * nb) // 4
            st = {}
            fe = {0: True, 1: True, 2: True}

            def stage1(it, b=b):
                nonlocal kk
                g, r, kb, nbk = it
                dil = DILS[g]
                nb = (S // dil) // 128
                Qs, rQs = (Qn[b], rQ[b]) if g == 0 else (Qd[b][g - 1], rQd[b][g - 1])
                Ks, rKs = (Kn[b], rK[b]) if g == 0 else (Kd[b][g - 1], rKd[b][g - 1])
                qb = r * nb + kb
                m0 = qb * 128
                N = 256 if kb + 1 < nb else 128
                pS, rS = psS.next()
                ph.mm(pS[:, 0:N], Ks[:, m0:m0 + 128], Qs[:, m0:m0 + N], True, True, reads=[rKs, rQs], writes=[rS])
                Pt, rP = ptp.next()
                ph.act(Pt[:, 0:N], pS[:, 0:N], AF.Exp, reads=[rS], writes=[rP])
                ph.v("pool" if kk % 3 == 2 else "dve", "tensor_tensor", Pt[:, 0:N], Pt[:, 0:N], Td[b][:, g, 0:N], ALU.mult,
                     reads=[rP, rT[b]], writes=[rP])
                kk += 1
                st[it] = (Pt, rP, N)

            def stage2(it, b=b):
                g, r, kb, nbk = it
                dil = DILS[g]
                Ls = S // dil
                nb = Ls // 128
                qb = r * nb + kb
                Pt, rP, N = st.pop(it)
                bi = (nbk + qb // 4) % 3
                co = (qb % 4) * 128
                ph.mm(obanks[bi][:, co:co + 128], Vd[b][g][:, qb * 65:qb * 65 + 128], Pt[:, 0:128], kb == 0, True,
                      reads=[rV[b], rP], writes=[rOb[bi]])
                if N == 256:
                    bi2 = (nbk + (qb + 1) // 4) % 3
                    co2 = ((qb + 1) % 4) * 128
                    ph.mm(obanks[bi2][:, co2:co2 + 128], Vd[b][g][:, qb * 65:qb * 65 + 128], Pt[:, 128:256], True, False,
                          reads=[rV[b], rP], writes=[rOb[bi2]])
                if qb % 4 == 3:
                    M0 = (qb // 4) * 512
                    bank = obanks[bi]
                    if g == 0:
                        ph.add("act", lambda e, bank=bank, M0=M0, b=b: e.activation(out=Oacc[b][0:65, M0:M0 + 512], in_=bank[0:65, :],
                                                                                  func=AF.Copy), [rOb[bi]], [rOa[b]], nowaw=not fe[g])
                    else:
                        ov = Oacc[b][0:65, :].rearrange("p (n r) -> p r n", r=dil)
                        r0, n0 = M0 // Ls, M0 % Ls
                        if Ls >= 512:
                            dst = ov[:, r0, n0:n0 + 512]
                            src = bank[0:65, :]
                        else:
                            a = 512 // Ls
                            dst = ov[:, r0:r0 + a, 0:Ls]
                            src = bank[0:65, :].rearrange("p (a n) -> p a n", a=a)
                        ph.add("dve", lambda e, dst=dst, src=src: e.tensor_tensor(dst, dst, src, ALU.add), [rOb[bi], rOa[b]], [rOa[b]],
                               nowaw=not fe[g])
                    fe[g] = False

            for i in range(len(items) + LA):
                if i < len(items):
                    stage1(items[i])
                if i >= LA:
                    stage2(items[i - LA])
                if chunks and i % 8 == 5:
                    chunks.pop(0)()
            while chunks:
                chunks.pop(0)()
            for qt in range(NT):
                self.finish_chunk(ph, None, None, Oacc[b], rOa[b], qt, h, psL, rinvp, osbp, lo, first=False, src_is_sbuf=True)

        prep_loads(0)
        for c_ in prep_chunks(0):
            c_()
        for h in range(16):
            nxt = []
            if h + 1 < 16:
                prep_loads(h + 1)
                nxt = prep_chunks(h + 1)
            sweep(h, nxt)
        ph.emit()


Prog.ph_dil = ph_dil


NBK = 23
BK = 512
NSLOT = NBK * BK
I32 = mybir.dt.int32


def ph_moe(self):
    nc, K, I, Dm = self.nc, self.K, self.I, self.Dm
    layer = 1
    hv = Dm["hT3"].rearrange("(c p) t -> p c t", p=128)
    xg = self.dscr("xg", [NSLOT, D], BF16)
    yg = self.dscr("yg", [NSLOT, D], F32)
    htm_d = self.dscr("h3tm", [S, D], F32)
    es0 = self.es
    idxG = self.sb(es0, "idxG", [128, 32, 2], I32)
    pk = self.sb(es0, "pk", [128, 32, 2], F32)
    idxW = self.sb(es0, "idxW", [128, NBK, 32], I32)
    idxD = self.sb(es0, "idxD", [128, NBK, 28], I32)
    SW = 256
    with ExitStack() as es:
        ph = Phase(K, "moe_a")
        wk = self.make_wk(es, ph, width=SW)
        l0 = K.lane()
        Wr, rWr = self.load_weight_bf16(ph, es, "Wr", I["router"], D, 8, K.lane())
        tri = self.sb(es, "tri", [128, 128], BF16)
        onesb = self.sb(es, "onesb", [128, 128], BF16)
        c1 = self.sb(es, "c1", [128, 32], F32)
        c2 = self.sb(es, "c2", [128, 28], F32)
        jv = self.sb(es, "jv", [128, NBK], F32)
        rc = Res()
        ph.dma("sp", tri[:], I["tri"], l0, writes=[rc], nowaw=True)
        ph.dma("sp", c1[:], I["c1"], l0, writes=[rc], nowaw=True)
        ph.dma("sp", c2[:], I["c2"], l0, writes=[rc], nowaw=True)
        ph.dma("sp", jv[:], I["jv"][:, 0:NBK], l0, writes=[rc], nowaw=True)
        ph.v("pool", "memset", onesb[:], 1.0, writes=[rc], nowaw=True)
        carry = self.sb(es, "carry", [128, 8], F32)
        rcar = Res()
        ph.v("dve", "memset", carry[:], 0.0, writes=[rcar])
        htp = Pool2([self.sb(es, "hts%d" % i, [128, 8, SW], F32) for i in range(2)])
        lh = [K.lane(), K.lane()]
        utp = Pool2([self.sb(es, "uts%d" % i, [128, 8, SW], BF16) for i in range(2)])
        utm_all = self.sb(es, "utm_all", [128, 32, D], BF16)
        rum = Res()
        lsc = [K.lane(), K.lane()]
        htm_p = Pool2([self.sb(es, "htm%d" % i, [128, D], F32) for i in range(2)])
        lht = [K.lane(), K.lane()]
        psU = Pool2([self.ps(es, "psU%d" % i, [128, D], BF16) for i in range(2)])
        psH = Pool2([self.ps(es, "psH%d" % i) for i in range(2)])
        psR = Pool2([self.ps(es, "psR")])
        psP = Pool2([self.ps(es, "psP")])
        lg = self.sb(es, "lg", [128, 8], F32)
        mx = self.sb(es, "mx", [128, 8], F32)
        dd = self.sb(es, "dd", [128, 1], F32)
        mm2 = self.sb(es, "mm2", [128, 32, 2, 8], F32)
        Mb = self.sb(es, "Mb", [128, 8], BF16)
        posf = self.sb(es, "posf", [128, 32, 8], F32)
        rl = Res()
        rpos = Res()
        def load_h(sub):
            ht, rh = htp.next()
            ph.dma("sp", ht[:], hv[:, :, sub * SW:(sub + 1) * SW], lh[sub % 2], writes=[rh])
            return ht, rh

        nxt_h = load_h(0)
        for sub in range(S // SW):
            tok = sub * SW
            ht, rh = nxt_h
            if sub + 1 < S // SW:
                nxt_h = load_h(sub + 1)
            ut, ru = utp.next()
            self.norm_modulate(ph, ht[:], rh, layer, 2, ut[:], ru, wk, width=SW)
            for s2 in range(SW // 128):
                i = (tok // 128) + s2
                cs = slice(s2 * 128, (s2 + 1) * 128)
                htm, rhm = htm_p.next()
                for hh in range(2):
                    pX, rX = psH.next()
                    for c4 in range(4):
                        ph.tr(pX[:, c4 * 128:(c4 + 1) * 128], ht[:, hh * 4 + c4, cs], self.ident_f[:], reads=[rh], writes=[rX])
                    if hh == 0:
                        ph.add("act", lambda e, htm=htm, pX=pX: e.activation(out=htm[:, 0:512], in_=pX[:], func=AF.Copy), [rX], [rhm])
                    else:
                        ph.add("dve", lambda e, htm=htm, pX=pX: e.tensor_copy(htm[:, 512:1024], pX[:]), [rX], [rhm], nowaw=True)
                ph.dma("sp", htm_d[i * 128:(i + 1) * 128, :], htm[:], lht[i % 2], reads=[rhm])
                pU, rU = psU.next()
                for c in range(8):
                    ph.tr(pU[:, c * 128:(c + 1) * 128], ut[:, c, cs], self.ident_b[:], reads=[ru], writes=[rU])
                ph.add("act", lambda e, i=i, pU=pU: e.activation(out=utm_all[:, i, :], in_=pU[:], func=AF.Copy), [rU], [rum], nowaw=True)
                pR, rR = psR.next()
                for c in range(8):
                    ph.mm(pR[:, 0:8], ut[:, c, cs], Wr[:, c, 0:8], c == 0, c == 7, reads=[ru, rWr], writes=[rR])
                ph.v("dve", "tensor_copy", lg[:], pR[:, 0:8], reads=[rR], writes=[rl])
                ph.v("dve", "max", mx[:], lg[:], reads=[rl], writes=[rl])
                ph.v("dve", "tensor_tensor", dd[:], mx[:, 0:1], mx[:, 1:2], ALU.subtract, reads=[rl], writes=[rl])
                ph.add("act", lambda e, i=i: e.activation(out=pk[:, i, 0:1], in_=dd[:], func=AF.Sigmoid), [rl], [rl])
                ph.add("act", lambda e, i=i: e.activation(out=pk[:, i, 1:2], in_=dd[:], func=AF.Sigmoid, scale=-1.0), [rl], [rl])
                ph.add("dve", lambda e, i=i: e.tensor_scalar(mm2[:, i, 0, :], lg[:], mx[:, 0:1], None, ALU.is_ge), [rl], [rl])
                ph.add("dve", lambda e, i=i: e.tensor_scalar(mm2[:, i, 1, :], lg[:], mx[:, 1:2], None, ALU.is_equal), [rl], [rl])
                ph.add("dve", lambda e, i=i: e.tensor_tensor(Mb[:], mm2[:, i, 0, :], mm2[:, i, 1, :], ALU.add), [rl], [rl])
                pP, rP = psP.next()
                ph.mm(pP[:, 0:8], tri[:], Mb[:], True, True, reads=[rl, rc], writes=[rP])
                ph.mm(pP[:, 8:16], onesb[:], Mb[:], True, True, reads=[rl, rc], writes=[rP])
                ph.add("dve", lambda e, i=i, pP=pP: e.tensor_tensor(posf[:, i, :], pP[:, 0:8], carry[:], ALU.add), [rP, rcar], [rpos], nowaw=True)
                ph.add("dve", lambda e, pP=pP: e.tensor_tensor(carry[:], carry[:], pP[:, 8:16], ALU.add), [rP, rpos], [rcar])
        nbk = self.sb(es, "nbk", [128, 8], F32)
        cum = self.sb(es, "cum", [128, 8], F32)
        off = self.sb(es, "off", [128, 8], F32)
        rb = Res()
        ph.v("dve", "tensor_scalar", nbk[:], carry[:], 0.0, None, ALU.is_gt, reads=[rcar], writes=[rb])
        for m in range(1, 8):
            ph.v("dve", "scalar_tensor_tensor", nbk[:], carry[:], float(BK * m), nbk[:], ALU.is_gt, ALU.add, reads=[rcar, rb], writes=[rb])
        ph.v("dve", "tensor_copy", cum[:, 0:1], nbk[:, 0:1], reads=[rb], writes=[rb])
        for e_ in range(1, 8):
            ph.v("dve", "tensor_tensor", cum[:, e_:e_ + 1], cum[:, e_ - 1:e_], nbk[:, e_:e_ + 1], ALU.add, reads=[rb], writes=[rb])
        ph.v("dve", "tensor_tensor", off[:], cum[:], nbk[:], ALU.subtract, reads=[rb], writes=[rb])
        ph.v("dve", "tensor_scalar", off[:], off[:], float(BK), None, ALU.mult, reads=[rb], writes=[rb])
        cmp_ = self.sb(es, "cmp", [128, NBK, 8], F32)
        eb = self.sb(es, "eb", [128, NBK], F32)
        ph.v("dve", "tensor_tensor", cmp_[:], cum[:, None, :].to_broadcast([128, NBK, 8]), jv[:, 0:NBK, None].to_broadcast([128, NBK, 8]),
             ALU.is_le, reads=[rb, rc], writes=[rb])
        ph.v("dve", "tensor_reduce", eb[:], cmp_[:], AX.X, ALU.add, reads=[rb], writes=[rb])
        ph.v("dve", "tensor_scalar", eb[:], eb[:], 7.0, None, ALU.min, reads=[rb], writes=[rb])
        ebw = self.sb(es, "ebw", [128, NBK, 2], F32)
        ph.add("dve", lambda e: e.tensor_scalar(ebw[:, :, 0], eb[:], 4096.0, None, ALU.mult), [rb], [rb])
        ph.add("dve", lambda e: e.tensor_scalar(ebw[:, :, 1], eb[:], 3584.0, None, ALU.mult), [rb], [rb])
        ridx = Res()
        ph.v("dve", "tensor_tensor", idxW[:], c1[:, None, :].to_broadcast([128, NBK, 32]), ebw[:, :, 0:1].to_broadcast([128, NBK, 32]),
             ALU.add, reads=[rb, rc], writes=[ridx])
        ph.v("dve", "tensor_tensor", idxD[:], c2[:, None, :].to_broadcast([128, NBK, 28]), ebw[:, :, 1:2].to_broadcast([128, NBK, 28]),
             ALU.add, reads=[rb, rc], writes=[ridx], nowaw=True)
        sfa = self.sb(es, "sfa", [128, 32, 8], F32)
        t1a = self.sb(es, "t1a", [128, 32, 2, 8], F32)
        ssel = self.sb(es, "ssel", [128, 32, 2], F32)
        ph.v("dve", "tensor_tensor", sfa[:], posf[:], off[:, None, :].to_broadcast([128, 32, 8]), ALU.add, reads=[rpos, rb], writes=[rb])
        ph.v("dve", "tensor_tensor", t1a[:], mm2[:], sfa[:, :, None, :].to_broadcast([128, 32, 2, 8]), ALU.mult, reads=[rb, rl], writes=[rb])
        ph.v("dve", "tensor_reduce", ssel[:], t1a[:], AX.X, ALU.add, reads=[rb], writes=[rb])
        ph.v("dve", "tensor_copy", idxG[:], ssel[:], reads=[rb], writes=[ridx], nowaw=True)
        for i in range(32):
            for k in range(2):
                ph.add("pool", lambda e, i=i, k=k: e.indirect_dma_start(
                    out=xg[:, :], out_offset=bass.IndirectOffsetOnAxis(ap=idxG[:, i, k:k + 1], axis=0),
                    in_=utm_all[:, i, :], in_offset=None), [rum, ridx], [], lane=lsc[i % 2])
        if self.debug:
            dbc = self.dscr("dbg_counts", [128, 8], F32, dbg=True)
            ph.dma("sp", dbc, carry[:], l0, reads=[rcar])
        ph.emit()
    Wgv = I["exp_g"].rearrange("e k (g j) -> (e k g) j", j=896)
    Wuv = I["exp_u"].rearrange("e k (g j) -> (e k g) j", j=896)
    Wdv = I["exp_d"].rearrange("e f d -> (e f) d")
    with ExitStack() as es:
        ph = Phase(K, "moe_b")
        xT = [self.sb(es, "xT%d" % i, [128, 8, BK], BF16) for i in range(2)]
        rxT = [Res(), Res()]
        yaccp = [self.sb(es, "yacc%d" % i, [128, 8, BK], F32) for i in range(2)]
        wgp = [self.sb(es, "wg%d" % i, [128, 8, 896], BF16) for i in range(2)]
        wup = [self.sb(es, "wu%d" % i, [128, 8, 896], BF16) for i in range(2)]
        wdp = [self.sb(es, "wd%d" % i, [128, 7, D], BF16) for i in range(2)]
        rw = [Res(), Res()]
        lw = [K.lane(), K.lane()]
        hmp = Pool2([self.sb(es, "hmid%d" % i, [128, 7, BK], BF16) for i in range(2)])
        sp_ = Pool2([self.sb(es, "ssb%d" % i, [128, BK], BF16) for i in range(2)])
        xtl = Pool2([self.sb(es, "xtl%d" % i, [128, D], BF16) for i in range(3)])
        lx = [K.lane() for _ in range(3)]
        ytl = Pool2([self.sb(es, "ytl%d" % i, [128, D], F32) for i in range(2)])
        ly = [K.lane(), K.lane()]
        pa = Pool2([self.ps(es, "pa%d" % i) for i in range(2)])
        pbk = Pool2([self.ps(es, "pb%d" % i) for i in range(2)])
        pyk = Pool2([self.ps(es, "py%d" % i) for i in range(2)])
        pxu = Pool2([self.ps(es, "pxu", [128, D], BF16)])
        pyt = Pool2([self.ps(es, "pyt")])
        wcount = 0
        nx = 0

        def load_x(b_):
            nonlocal nx
            xb = xT[b_ % 2]
            first = True
            for j in range(BK // 128):
                xt_, rx_ = xtl.next()
                ph.dma("sp", xt_[:], xg[b_ * BK + j * 128: b_ * BK + (j + 1) * 128, :], lx[nx % 3], writes=[rx_])
                nx += 1
                pU, rU = pxu.next()
                for c in range(8):
                    ph.tr(pU[:, c * 128:(c + 1) * 128], xt_[:, c * 128:(c + 1) * 128], self.ident_b[:], reads=[rx_], writes=[rU])
                src = pU[:].rearrange("p (c s) -> p c s", c=8)
                dst = xb[:, :, j * 128:(j + 1) * 128]
                ph.add("act", lambda e, dst=dst, src=src: e.activation(out=dst, in_=src, func=AF.Copy), [rU], [rxT[b_ % 2]], nowaw=not first)
                first = False

        def load_w(b_, g4):
            nonlocal wcount
            wb = wcount % 2
            wcount += 1
            first = True
            for c in range(8):
                for Wv, wt in ((Wgv, wgp[wb]), (Wuv, wup[wb])):
                    ph.add("pool", lambda e, wt=wt, Wv=Wv, b_=b_, c=c, g4=g4: e.indirect_dma_start(
                        out=wt[:, c, :], out_offset=None, in_=Wv[:, :],
                        in_offset=bass.IndirectOffsetOnAxis(ap=idxW[:, b_, c * 4 + g4:c * 4 + g4 + 1], axis=0)),
                        [], [rw[wb]], lane=lw[wb], nowaw=not first)
                    first = False
            for j in range(7):
                ph.add("pool", lambda e, wb=wb, b_=b_, j=j, g4=g4: e.indirect_dma_start(
                    out=wdp[wb][:, j, :], out_offset=None, in_=Wdv[:, :],
                    in_offset=bass.IndirectOffsetOnAxis(ap=idxD[:, b_, g4 * 7 + j:g4 * 7 + j + 1], axis=0)),
                    [], [rw[wb]], lane=lw[wb], nowaw=True)
            return wb

        rydp = [[Res() for _ in range(8)] for _ in range(2)]
        load_x(0)
        pending = load_w(0, 0)
        for b_ in range(NBK):
            xb, rxb = xT[b_ % 2], rxT[b_ % 2]
            yacc = yaccp[b_ % 2]
            ryd = rydp[b_ % 2]
            for g4 in range(4):
                wb = pending
                if g4 < 3:
                    pending = load_w(b_, g4 + 1)
                elif b_ + 1 < NBK:
                    pending = load_w(b_ + 1, 0)
                if g4 == 1 and b_ + 1 < NBK:
                    load_x(b_ + 1)
                wg, wu, wd = wgp[wb], wup[wb], wdp[wb]
                hmid, rhm_ = hmp.next()
                for j in range(7):
                    pA, rA = pa.next()
                    pB, rB = pbk.next()
                    for c in range(8):
                        ph.mm(pA[:], wg[:, c, j * 128:(j + 1) * 128], xb[:, c, :], c == 0, c == 7, reads=[rw[wb], rxb], writes=[rA])
                    for c in range(8):
                        ph.mm(pB[:], wu[:, c, j * 128:(j + 1) * 128], xb[:, c, :], c == 0, c == 7, reads=[rw[wb], rxb], writes=[rB])
                    ss, rs = sp_.next()
                    ph.act(ss[:], pA[:], AF.Silu, reads=[rA], writes=[rs])
                    ph.add("dve", lambda e, j=j, ss=ss, pB=pB, hmid=hmid: e.tensor_tensor(hmid[:, j, :], ss[:], pB[:], ALU.mult),
                           [rs, rB], [rhm_], nowaw=(j > 0))
                for dc in range(8):
                    pY, rY = pyk.next()
                    for j in range(7):
                        ph.mm(pY[:], wd[:, j, dc * 128:(dc + 1) * 128], hmid[:, j, :], j == 0, j == 6, reads=[rw[wb], rhm_], writes=[rY])
                    if g4 == 0:
                        ph.add("act", lambda e, dc=dc, pY=pY, yacc=yacc: e.activation(out=yacc[:, dc, :], in_=pY[:], func=AF.Copy), [rY], [ryd[dc]])
                    else:
                        ph.add("dve", lambda e, dc=dc, pY=pY, yacc=yacc: e.tensor_tensor(yacc[:, dc, :], yacc[:, dc, :], pY[:], ALU.add),
                               [rY, ryd[dc]], [ryd[dc]])
            for j in range(BK // 128):
                yt_, ryt = ytl.next()
                for hh in range(2):
                    pX, rX = pyt.next()
                    for c4 in range(4):
                        c = hh * 4 + c4
                        ph.tr(pX[:, c4 * 128:(c4 + 1) * 128], yacc[:, c, j * 128:(j + 1) * 128], self.ident_f[:], reads=[ryd[c]], writes=[rX])
                    ph.add("act", lambda e, yt_=yt_, pX=pX, hh=hh: e.activation(out=yt_[:, hh * 512:(hh + 1) * 512], in_=pX[:], func=AF.Copy),
                           [rX], [ryt], nowaw=(hh > 0))
                ph.dma("sp", yg[b_ * BK + j * 128: b_ * BK + (j + 1) * 128, :], yt_[:], ly[j % 2], reads=[ryt])
        ph.emit()
    with ExitStack() as es:
        ph = Phase(K, "moe_c")
        selE = self.sb(es, "selE", [8, 8, 128], F32)
        rse = Res()
        ph.v("dve", "tensor_copy", selE[:], self.ident_f[0:8, 0:8, None].to_broadcast([8, 8, 128]), writes=[rse])
        pg = [self.ps(es, "pg%d" % i) for i in range(2)]
        rpg = Res()
        ggT = self.sb(es, "ggT", [8, 128], F32)
        rgt = Res()
        ph.tr(pg[0][0:8, 0:128], self.vecs[:, 1, 5, :], self.ident_f[:], writes=[rpg])
        ph.act(ggT[:], pg[0][0:8, 0:128], AF.Copy, reads=[rpg], writes=[rgt])
        ggbc = self.sb(es, "ggbc", [128, D], F32)
        rgb = Res()
        for c in range(8):
            ph.mm(pg[c // 4][:, (c % 4) * 128:(c % 4 + 1) * 128], selE[0:8, c, :], ggT[0:8, :], True, True, reads=[rse, rgt, rpg], writes=[rpg])
        for hh in range(2):
            ph.add("act", lambda e, hh=hh: e.activation(out=ggbc[:, hh * 512:(hh + 1) * 512], in_=pg[hh][:], func=AF.Copy), [rpg], [rgb], nowaw=(hh > 0))
        eps_t = self.sb(es, "eps_t", [128, 1], F32)
        ph.v("pool", "memset", eps_t[:], EPS, writes=[rgb], nowaw=True)
        yap = Pool2([self.sb(es, "ya%d" % i, [128, D], F32) for i in range(3)])
        ybp = Pool2([self.sb(es, "yb%d" % i, [128, D], F32) for i in range(3)])
        hmp2 = Pool2([self.sb(es, "hm%d" % i, [128, D], F32) for i in range(3)])
        jk = Pool2([self.sb(es, "jk%d" % i, [128, D], BF16) for i in range(2)])
        ssp = Pool2([self.sb(es, "ss%d" % i, [128, 2], F32) for i in range(2)])
        la, lb_, lh2, lo2 = ([K.lane(), K.lane(), K.lane()] for _ in range(4))
        def fetch(i):
            ya, rya = yap.next()
            yb, ryb = ybp.next()
            hm, rhm2 = hmp2.next()
            ph.add("pool", lambda e, i=i, ya=ya: e.indirect_dma_start(
                out=ya[:, :], out_offset=None, in_=yg[:, :], in_offset=bass.IndirectOffsetOnAxis(ap=idxG[:, i, 0:1], axis=0)),
                [], [rya], lane=la[i % 3])
            ph.add("pool", lambda e, i=i, yb=yb: e.indirect_dma_start(
                out=yb[:, :], out_offset=None, in_=yg[:, :], in_offset=bass.IndirectOffsetOnAxis(ap=idxG[:, i, 1:2], axis=0)),
                [], [ryb], lane=lb_[i % 3])
            ph.dma("sp", hm[:], htm_d[i * 128:(i + 1) * 128, :], lh2[i % 3], writes=[rhm2])
            return ya, rya, yb, ryb, hm, rhm2

        fq = [fetch(0), fetch(1)]
        for i in range(32):
            ya, rya, yb, ryb, hm, rhm2 = fq.pop(0)
            if i + 2 < 32:
                fq.append(fetch(i + 2))
            ph.add("dve", lambda e, i=i, ya=ya: e.tensor_scalar(ya[:], ya[:], pk[:, i, 0:1], None, ALU.mult), [rya], [rya])
            ph.add("dve", lambda e, i=i, ya=ya, yb=yb: e.scalar_tensor_tensor(ya[:], yb[:], pk[:, i, 1:2], ya[:], ALU.mult, ALU.add),
                   [rya, ryb], [rya])
            j_, rj = jk.next()
            ss, rss = ssp.next()
            ph.add("act", lambda e, ya=ya, j_=j_, ss=ss: e.activation(out=j_[:], in_=ya[:], func=AF.Square, accum_out=ss[:, 0:1]),
                   [rya], [rj, rss])
            ph.add("act", lambda e, ss=ss: e.activation(out=ss[:, 1:2], in_=ss[:, 0:1], func=AF.Sqrt, bias=eps_t[:, 0:1], scale=1.0 / D),
                   [rss, rgb], [rss])
            ph.add("dve", lambda e, ss=ss: e.reciprocal(ss[:, 1:2], ss[:, 1:2]), [rss], [rss])
            ph.add("dve", lambda e, ya=ya, ss=ss: e.scalar_tensor_tensor(ya[:], ya[:], ss[:, 1:2], ggbc[:], ALU.mult, ALU.mult),
                   [rya, rss, rgb], [rya])
            ph.add("pool", lambda e, ya=ya, hm=hm: e.tensor_tensor(hm[:], hm[:], ya[:], ALU.add), [rya, rhm2], [rhm2])
            ph.dma("sp", self.out[i * 128:(i + 1) * 128, :], hm[:], lo2[i % 3], reads=[rhm2])
        ph.emit()


Prog.ph_moe = ph_moe
```
